# Optimizing a Trainium2 kernel written in Bass

```python
import math
import jax, jax.numpy as jnp
from jax import lax
import numpy as np

D_MODEL = 2048
BATCH = 4
SEQ = 4096
DEPTH = 1

GRID_W = 64
CTX_LEN = 256
EPS = 1e-6
HEAD_DIM = 128
N_Q_HEADS = 8
N_KV_HEADS = 2
GROUP = N_Q_HEADS // N_KV_HEADS
ATT_WIDTH = N_Q_HEADS * HEAD_DIM
KV_WIDTH = N_KV_HEADS * HEAD_DIM
ROPE_THETA = 10000.0
ROPE_FREQS = HEAD_DIM // 4
Q_BLOCK = 128
REC_WIDTH = D_MODEL - ATT_WIDTH
REC_BLOCKS = 8
REC_BLOCK_DIM = REC_WIDTH // REC_BLOCKS
CONV_W = 4
CONV_LEFT = 2
LRU_C = 8.0
IN_WIDTH = ATT_WIDTH + 2 * KV_WIDTH + 2 * REC_WIDTH
SPLITS = (ATT_WIDTH, ATT_WIDTH + KV_WIDTH, ATT_WIDTH + 2 * KV_WIDTH, ATT_WIDTH + 2 * KV_WIDTH + REC_WIDTH)
N_EXPERTS = 32
TOP_K = 4
D_FF = D_MODEL
SWIGLU_LIMIT = 7.0
SWIGLU_ALPHA = 1.702
MOE_BLOCK = 256

kernel_name = "hybrid_gqa_rglru_moe_dit_layer"


def rmsnorm(x, g):
    xf = x.astype(jnp.float32)
    y = xf * lax.rsqrt(jnp.mean(xf * xf, axis=-1, keepdims=True) + EPS)
    return y.astype(x.dtype) * g


def modulate(x, shift, scale):
    return x * (1 + scale) + shift


def axial_rope(seq):
    rows = seq // GRID_W
    row = jnp.repeat(jnp.arange(rows), GRID_W).astype(jnp.float32)
    col = jnp.tile(jnp.arange(GRID_W), rows).astype(jnp.float32)
    inv = ROPE_THETA ** (-jnp.arange(ROPE_FREQS, dtype=jnp.float32) / ROPE_FREQS)
    ang = jnp.stack([row[:, None] * inv, col[:, None] * inv], axis=1)
    return jnp.cos(ang), jnp.sin(ang)


def apply_rope(x, cos, sin):
    xs = x.reshape(*x.shape[:-1], 2, 2, ROPE_FREQS)
    x1, x2 = xs[..., 0, :], xs[..., 1, :]
    c = cos[:, None].astype(x.dtype)
    s = sin[:, None].astype(x.dtype)
    return jnp.stack([x1 * c - x2 * s, x2 * c + x1 * s], axis=-2).reshape(x.shape)


def attend(q, k, v):
    s = jnp.einsum('bqkgd,bnkd->bkgqn', q, k).astype(jnp.float32) * (HEAD_DIM ** -0.5)
    p = jax.nn.softmax(s, axis=-1).astype(v.dtype)
    return jnp.einsum('bkgqn,bnkd->bqkgd', p, v)


def short_conv(u, w, b):
    L = u.shape[1]
    up = jnp.pad(u, ((0, 0), (CONV_LEFT, CONV_W - 1 - CONV_LEFT), (0, 0)))
    out = b
    for j in range(CONV_W):
        out = out + up[:, j:j + L] * w[j]
    return out


def lru_coeffs(u, w_a, b_a, w_x, b_x, lam):
    Bsz, L, _ = u.shape
    ub = u.reshape(Bsz, L, REC_BLOCKS, REC_BLOCK_DIM)
    ga = jnp.einsum('blnd,rnde->rblne', ub, w_a).reshape(2, Bsz, L, REC_WIDTH) + b_a[:, None, None, :]
    gx = jnp.einsum('blnd,rnde->rblne', ub, w_x).reshape(2, Bsz, L, REC_WIDTH) + b_x[:, None, None, :]
    r = jax.nn.sigmoid(ga.astype(jnp.float32))
    i = jax.nn.sigmoid(gx.astype(jnp.float32))
    log_a = -LRU_C * r * jax.nn.softplus(-lam.astype(jnp.float32))[:, None, None, :]
    a = jnp.exp(log_a)
    mult = jnp.sqrt(-jnp.expm1(2.0 * log_a))
    return a, mult * i * u.astype(jnp.float32)[None]


def linear_scan(a, b, h0):
    b = b.at[:, 0].add(a[:, 0] * h0)

    def comb(left, right):
        a_l, b_l = left
        a_r, b_r = right
        return a_r * a_l, a_r * b_l + b_r

    _, h = lax.associative_scan(comb, (a, b), axis=1)
    return h


def bidir_scan(a, b, h0_f, h0_b):
    h_f = linear_scan(a[0], b[0], h0_f)
    h_b = jnp.flip(linear_scan(jnp.flip(a[1], 1), jnp.flip(b[1], 1), h0_b), 1)
    return h_f, h_b


def moe(h, w_router, b_router, w_gu, b_gu, w_down, b_down):
    T, D = h.shape
    logits = (h @ w_router + b_router).astype(jnp.float32)
    top_val, top_idx = lax.top_k(logits, TOP_K)
    gate = jax.nn.softmax(top_val, axis=-1)
    A = T * TOP_K
    e_flat = top_idx.reshape(A)
    tok_flat = (jnp.arange(A, dtype=jnp.int32) // TOP_K).astype(jnp.int32)
    w_flat = gate.reshape(A)
    order = jnp.argsort(e_flat)
    e_sorted, tok_sorted, w_sorted = e_flat[order], tok_flat[order], w_flat[order]
    counts = jnp.zeros((N_EXPERTS,), jnp.int32).at[e_flat].add(1)
    starts = jnp.cumsum(counts) - counts
    padded = (counts + MOE_BLOCK - 1) // MOE_BLOCK * MOE_BLOCK
    pad_starts = jnp.cumsum(padded) - padded
    pad_ends = pad_starts + padded
    dest = pad_starts[e_sorted] + (jnp.arange(A, dtype=jnp.int32) - starts[e_sorted])
    n_blocks = (A + N_EXPERTS * (MOE_BLOCK - 1) + MOE_BLOCK - 1) // MOE_BLOCK
    P = n_blocks * MOE_BLOCK
    slot_tok = jnp.full((P,), T, jnp.int32).at[dest].set(tok_sorted)
    slot_w = jnp.zeros((P,), h.dtype).at[dest].set(w_sorted.astype(h.dtype))
    block_start = jnp.arange(n_blocks, dtype=jnp.int32) * MOE_BLOCK
    block_e = jnp.minimum(jnp.searchsorted(pad_ends, block_start, side='right'), N_EXPERTS - 1)
    h_pad = jnp.concatenate([h, jnp.zeros((1, D), h.dtype)], axis=0)

    def expert_block(args):
        e, toks, w = args
        xb = h_pad[toks]
        gu = xb @ w_gu[e] + b_gu[e]
        g_, up = gu[:, 0::2], gu[:, 1::2]
        g_ = jnp.minimum(g_, SWIGLU_LIMIT)
        up = jnp.clip(up, -SWIGLU_LIMIT, SWIGLU_LIMIT)
        act = (up + 1) * (g_ * jax.nn.sigmoid(SWIGLU_ALPHA * g_))
        return (act @ w_down[e] + b_down[e]) * w[:, None]

    outs = lax.map(expert_block, (block_e, slot_tok.reshape(n_blocks, MOE_BLOCK), slot_w.reshape(n_blocks, MOE_BLOCK)))
    y = jnp.zeros((T + 1, D), h.dtype).at[slot_tok].add(outs.reshape(P, D))
    return y[:T]


def setup_inputs(seed: int = 0) -> dict:
    key = jax.random.key(seed)
    ks = jax.random.split(key, 32)
    f32 = jnp.float32
    D = D_MODEL

    def nrm(k, shape, scale):
        return jax.random.normal(k, shape, f32) * scale

    u = jax.random.uniform(ks[16], (DEPTH, 2, REC_WIDTH), f32, 0.9, 0.999)
    return {
        "x": nrm(ks[0], (BATCH, SEQ, D), 1.0),
        "c": nrm(ks[1], (BATCH, D), 1.0),
        "ctx": nrm(ks[2], (BATCH, CTX_LEN, D), 1.0),
        "c_ctx": nrm(ks[3], (D,), 1.0),
        "w_mod": nrm(ks[4], (DEPTH, D, 6 * D), 0.5 * D ** -0.5),
        "b_mod": nrm(ks[5], (DEPTH, 6 * D), 0.02),
        "g_norm1": 1.0 + nrm(ks[6], (DEPTH, D), 0.02),
        "w_in": nrm(ks[7], (DEPTH, D, IN_WIDTH), D ** -0.5),
        "g_q": 1.0 + nrm(ks[8], (DEPTH, HEAD_DIM), 0.02),
        "g_k": 1.0 + nrm(ks[9], (DEPTH, HEAD_DIM), 0.02),
        "conv_w": nrm(ks[10], (DEPTH, CONV_W, REC_WIDTH), CONV_W ** -0.5),
        "conv_b": nrm(ks[11], (DEPTH, REC_WIDTH), 0.02),
        "w_gate_a": nrm(ks[12], (DEPTH, 2, REC_BLOCKS, REC_BLOCK_DIM, REC_BLOCK_DIM), REC_BLOCK_DIM ** -0.5),
        "b_gate_a": nrm(ks[13], (DEPTH, 2, REC_WIDTH), 0.02),
        "w_gate_x": nrm(ks[14], (DEPTH, 2, REC_BLOCKS, REC_BLOCK_DIM, REC_BLOCK_DIM), REC_BLOCK_DIM ** -0.5),
        "b_gate_x": nrm(ks[15], (DEPTH, 2, REC_WIDTH), 0.02),
        "lru_lambda": jnp.log(u) - jnp.log1p(-u),
        "g_att_out": 1.0 + nrm(ks[17], (DEPTH, ATT_WIDTH), 0.02),
        "g_rec_out": 1.0 + nrm(ks[18], (DEPTH, REC_WIDTH), 0.02),
        "w_out": nrm(ks[19], (DEPTH, D, D), D ** -0.5),
        "g_norm2": 1.0 + nrm(ks[20], (DEPTH, D), 0.02),
        "w_router": nrm(ks[21], (DEPTH, D, N_EXPERTS), D ** -0.5),
        "b_router": nrm(ks[22], (DEPTH, N_EXPERTS), 0.01),
        "w_gate_up": nrm(ks[23], (DEPTH, N_EXPERTS, D, 2 * D_FF), D ** -0.5),
        "b_gate_up": nrm(ks[24], (DEPTH, N_EXPERTS, 2 * D_FF), 0.02),
        "w_down": nrm(ks[25], (DEPTH, N_EXPERTS, D_FF, D), D_FF ** -0.5),
        "b_down": nrm(ks[26], (DEPTH, N_EXPERTS, D), 0.02),
    }


def reference(x, c, ctx, c_ctx, w_mod, b_mod, g_norm1, w_in, g_q, g_k, conv_w, conv_b,
              w_gate_a, b_gate_a, w_gate_x, b_gate_x, lru_lambda, g_att_out, g_rec_out, w_out,
              g_norm2, w_router, b_router, w_gate_up, b_gate_up, w_down, b_down):
    B, S, D = x.shape
    C = ctx.shape[1]
    cos, sin = axial_rope(S)
    n_qb = S // Q_BLOCK
    cx = ctx
    for l in range(DEPTH):
        update_ctx = l < DEPTH - 1
        mod = jax.nn.silu(c) @ w_mod[l] + b_mod[l]
        mod_c = jax.nn.silu(c_ctx) @ w_mod[l] + b_mod[l]
        sh1, sc1, gt1, sh2, sc2, gt2 = jnp.split(mod[:, None, :], 6, axis=-1)
        csh1, csc1, cgt1, csh2, csc2, cgt2 = jnp.split(mod_c, 6)

        h = modulate(rmsnorm(x, g_norm1[l]), sh1, sc1)
        hc = modulate(rmsnorm(cx, g_norm1[l]), csh1, csc1)
        q, k, v, xr, yr = jnp.split(h @ w_in[l], SPLITS, axis=-1)
        qc, kc, vc, xrc, yrc = jnp.split(hc @ w_in[l], SPLITS, axis=-1)

        q = apply_rope(rmsnorm(q.reshape(B, S, N_Q_HEADS, HEAD_DIM), g_q[l]), cos, sin)
        k = apply_rope(rmsnorm(k.reshape(B, S, N_KV_HEADS, HEAD_DIM), g_k[l]), cos, sin)
        v = v.reshape(B, S, N_KV_HEADS, HEAD_DIM)
        kc = rmsnorm(kc.reshape(B, C, N_KV_HEADS, HEAD_DIM), g_k[l])
        vc = vc.reshape(B, C, N_KV_HEADS, HEAD_DIM)
        k_all = jnp.concatenate([kc, k], axis=1)
        v_all = jnp.concatenate([vc, v], axis=1)
        qb = q.reshape(B, n_qb, Q_BLOCK, N_KV_HEADS, GROUP, HEAD_DIM).swapaxes(0, 1)
        att = lax.map(lambda qblk: attend(qblk, k_all, v_all), qb)
        att = att.swapaxes(0, 1).reshape(B, S, ATT_WIDTH)

        ac, bc = lru_coeffs(short_conv(xrc, conv_w[l], conv_b[l]), w_gate_a[l], b_gate_a[l],
                            w_gate_x[l], b_gate_x[l], lru_lambda[l])
        zeros = jnp.zeros((B, REC_WIDTH), jnp.float32)
        hcf, hcb = bidir_scan(ac, bc, zeros, zeros)
        a, bb = lru_coeffs(short_conv(xr, conv_w[l], conv_b[l]), w_gate_a[l], b_gate_a[l],
                           w_gate_x[l], b_gate_x[l], lru_lambda[l])
        hf, hb = bidir_scan(a, bb, hcf[:, -1], hcb[:, 0])
        rec = (hf + hb).astype(x.dtype) * jax.nn.gelu(yr, approximate=True)

        mix = jnp.concatenate([rmsnorm(att, g_att_out[l]), rmsnorm(rec, g_rec_out[l])], axis=-1) @ w_out[l]
        x = x + gt1 * mix
        h2 = modulate(rmsnorm(x, g_norm2[l]), sh2, sc2)
        x = x + gt2 * moe(h2.reshape(B * S, D), w_router[l], b_router[l], w_gate_up[l], b_gate_up[l],
                          w_down[l], b_down[l]).reshape(B, S, D)

        if update_ctx:
            qc = rmsnorm(qc.reshape(B, C, N_Q_HEADS, HEAD_DIM), g_q[l]).reshape(B, C, N_KV_HEADS, GROUP, HEAD_DIM)
            attc = attend(qc, kc, vc).reshape(B, C, ATT_WIDTH)
            recc = (hcf + hcb).astype(cx.dtype) * jax.nn.gelu(yrc, approximate=True)
            mixc = jnp.concatenate([rmsnorm(attc, g_att_out[l]), rmsnorm(recc, g_rec_out[l])], axis=-1) @ w_out[l]
            cx = cx + cgt1 * mixc
            h2c = modulate(rmsnorm(cx, g_norm2[l]), csh2, csc2)
            cx = cx + cgt2 * moe(h2c.reshape(B * C, D), w_router[l], b_router[l], w_gate_up[l], b_gate_up[l],
                                 w_down[l], b_down[l]).reshape(B, C, D)
    return x
```

```python
import numpy as np
import ml_dtypes
from contextlib import ExitStack
import concourse.bass as bass
import concourse.mybir as mybir
from concourse.bass_utils import run_bass_kernel_spmd

F32 = mybir.dt.float32
BF16 = mybir.dt.bfloat16
AF = mybir.ActivationFunctionType
ALU = mybir.AluOpType

D = 2048
SEQ = 4096
TOWN = 2048
CTX = 256
NTOK = CTX + SEQ
NE = 32
EPS = 1e-6
NCORES = 8

SEM_ROT = 30000


class Buf:
    __slots__ = ("name", "last_w", "readers", "dsem", "dcount")

    def __init__(self, name):
        self.name = name
        self.last_w = None
        self.readers = []
        self.dsem = None
        self.dcount = 0


class Op:
    __slots__ = ("eng", "fn", "deps", "dma", "dbuf", "need", "sem", "val")

    def __init__(self, eng, fn, dma, dbuf):
        self.eng = eng
        self.fn = fn
        self.deps = []
        self.dma = dma
        self.dbuf = dbuf
        self.need = False
        self.sem = None
        self.val = 0


class Sched:
    ENGS = ("pe", "act", "dve", "pool", "sp")

    def __init__(self, nc):
        self.nc = nc
        self.ops = []
        self.last = {e: None for e in self.ENGS}
        self.open_dmas = []

    def op(self, eng, fn, reads=(), writes=()):
        o = Op(eng, fn, False, None)
        self._deps(o, reads, writes)
        self.ops.append(o)
        self.last[eng] = o
        return o

    def dma(self, eng, out, in_, reads=(), writes=(), **kw):
        o = Op(eng, lambda e: e.dma_start(out=out, in_=in_, **kw), True, writes[0])
        self._deps(o, reads, writes)
        self.ops.append(o)
        self.open_dmas.append(o)
        return o

    def _deps(self, o, reads, writes):
        deps = o.deps
        for b in reads:
            w = b.last_w
            if w is not None:
                deps.append(w)
        for b in writes:
            w = b.last_w
            if w is not None and (w.dma or o.dma or w.eng != o.eng):
                deps.append(w)
            for r in b.readers:
                if r.dma or o.dma or r.eng != o.eng:
                    deps.append(r)
        for b in reads:
            b.readers.append(o)
        for b in writes:
            b.last_w = o
            b.readers = []

    def barrier(self):
        lasts = [o for o in self.last.values() if o is not None]
        dmas = self.open_dmas
        self.open_dmas = []
        news = []
        for e in self.ENGS:
            o = Op(e, lambda en: en.nop(), False, None)
            o.deps = [x for x in lasts if x.eng != e] + list(dmas)
            news.append(o)
        news[0].dbuf = "BARRIER"
        for o in news:
            self.ops.append(o)
            self.last[o.eng] = o

    def emit(self):
        nc = self.nc
        ops = self.ops
        for o in ops:
            if o.dma:
                o.need = True
            for d in o.deps:
                d.need = True
        stack = ExitStack()
        cur = {e: [None, 0] for e in self.ENGS}
        nsem = [0]

        def new_sem(tag):
            nsem[0] += 1
            return stack.enter_context(nc.semaphore(f"s{nsem[0]}_{tag}"))

        free_d = []
        scount = {}
        active = []
        for o in ops:
            if o.dbuf == "BARRIER":
                for b in active:
                    free_d.append(b.dsem)
                    b.dsem = None
                active = []
            if not o.need:
                continue
            if o.dma:
                b = o.dbuf
                if b.dsem is None:
                    b.dsem = free_d.pop() if free_d else new_sem("d")
                    active.append(b)
                k_ = id(b.dsem)
                scount[k_] = scount.get(k_, 0) + 16
                o.sem, o.val = b.dsem, scount[k_]
            else:
                c = cur[o.eng]
                if c[0] is None or c[1] >= SEM_ROT:
                    c[0] = new_sem(o.eng)
                    c[1] = 0
                c[1] += 1
                o.sem, o.val = c[0], c[1]
        self.nsem = nsem[0]
        streams = {e: [] for e in self.ENGS}
        waited = {e: {} for e in self.ENGS}
        for o in ops:
            wl = {}
            wd = waited[o.eng]
            for d in o.deps:
                k = id(d.sem)
                if wd.get(k, 0) >= d.val:
                    continue
                if k not in wl or wl[k][1] < d.val:
                    wl[k] = (d.sem, d.val)
            for k, (s, v) in wl.items():
                wd[k] = v
            streams[o.eng].append((o, list(wl.values())))
        with nc.Block() as block:
            def mk(ename):
                def body(e):
                    for o, wl in streams[ename]:
                        for s, v in wl:
                            e.wait_ge(s, v)
                        ins = o.fn(e)
                        if o.need:
                            ins.then_inc(o.sem, 16 if o.dma else 1)
                return body
            block.tensor(mk("pe"))
            block.scalar(mk("act"))
            block.vector(mk("dve"))
            block.gpsimd(mk("pool"))
            block.sync(mk("sp"))
        stack.close()


class K:
    pass


def _mk(k):
    nc = k.nc

    def sb(st, name, shape, dt):
        return st.enter_context(nc.sbuf_tensor("s_" + name, shape, dt))

    def ps(st, name, shape, dt):
        return st.enter_context(nc.psum_tensor("p_" + name, shape, dt))
    k.sb, k.ps = sb, ps


def phase_mod(k):
    nc, S, sb, ps = k.nc, k.S, k.sb, k.ps
    d = k.d
    with ExitStack() as st:
        cT = sb(st, "cT", [128, 16, 2], F32)
        scT = sb(st, "scT", [128, 16, 2], F32)
        bm = sb(st, "bm", [128, 96], F32)
        pmod = ps(st, "pmod", [128, 96, 2], F32)
        ptr = ps(st, "ptr", [96, 128], F32)
        mrow = sb(st, "mrow", [96, 128], F32)
        B_c, B_sc, B_bm, B_pm, B_ptr, B_mrow = (Buf(n) for n in "c sc bm pm ptr mrow".split())
        NSL = 2
        GW = 4
        slabs = [sb(st, f"wm{i}", [128, 16, GW * 128], F32) for i in range(NSL)]
        B_sl = [Buf(f"wm{i}") for i in range(NSL)]
        S.dma("sp", cT[:], d["cT"], writes=[B_c])
        S.dma("sp", bm[:], d["bm"], writes=[B_bm])
        S.op("act", lambda e: e.activation(scT[:], cT[:], AF.Silu), reads=[B_c], writes=[B_sc])
        wm = d["w_mod"].rearrange("(kc p) f -> p kc f", p=128)
        for g in range(96 // GW):
            sl, B = slabs[g % NSL], B_sl[g % NSL]
            S.dma("sp", sl[:], wm[:, :, g * GW * 128:(g + 1) * GW * 128], writes=[B])
            for fl in range(GW):
                fc = g * GW + fl
                for kc in range(16):
                    S.op("pe", lambda e, sl=sl, fl=fl, kc=kc, fc=fc: e.matmul(
                        pmod[:, fc, :], lhsT=sl[:, kc, fl * 128:(fl + 1) * 128], rhs=scT[:, kc, :],
                        start=(kc == 0), stop=(kc == 15)), reads=[B, B_sc], writes=[B_pm])
        mod = k.mod
        for j in range(2):
            S.op("dve", lambda e, j=j: e.tensor_tensor(mod[:, :, j], pmod[:, :, j], bm[:], ALU.add),
                 reads=[B_pm, B_bm], writes=[k.B_mod])
        for j in range(2):
            S.op("dve", lambda e, j=j: e.scalar_tensor_tensor(
                k.A1[:, :, j], in0=mod[:, 16:32, j], scalar=1.0, in1=k.g1[:], op0=ALU.add, op1=ALU.mult),
                reads=[k.B_mod, k.B_g], writes=[k.B_A])
        S.op("dve", lambda e: e.scalar_tensor_tensor(
            k.A2[:], in0=mod[:, 64:80, 0], scalar=1.0, in1=k.g2[:], op0=ALU.add, op1=ALU.mult),
            reads=[k.B_mod, k.B_g], writes=[k.B_A])
        S.op("pe", lambda e: e.transpose(ptr[:], mod[:, :, 0], k.ident_f[:]),
             reads=[k.B_mod, k.B_const], writes=[B_ptr])
        S.op("act", lambda e: e.activation(mrow[:], ptr[:], AF.Identity), reads=[B_ptr], writes=[B_mrow])
        S.dma("sp", d["MODd"], mrow[:], reads=[B_mrow], writes=[Buf("modd")])
    S.barrier()


def phase_proj(k):
    nc, S, sb, ps = k.nc, k.S, k.sb, k.ps
    d = k.d
    with ExitStack() as st:
        win = sb(st, "win", [128, 16, 3584], BF16)
        B_win = [Buf(f"win{i}") for i in range(16)]
        for kc in range(16):
            S.dma("pool", win[:, kc, :], d["w_in"][kc * 128:(kc + 1) * 128, :], writes=[B_win[kc]])
        NX = 2
        xt = [sb(st, f"xt{i}", [128, D], F32) for i in range(NX)]
        B_xt = [Buf(f"xt{i}") for i in range(NX)]
        junk = sb(st, "junk", [128, D], BF16)
        B_junk = Buf("junk")
        stat = sb(st, "stat", [128, 3, 4], F32)
        B_ss = [Buf(f"ss{i}") for i in range(4)]
        B_sd = [Buf(f"sd{i}") for i in range(4)]
        B_rs = [Buf(f"rs{i}") for i in range(4)]
        xn = sb(st, "xn", [128, 4, D], BF16)
        B_xn = [Buf(f"xn{i}") for i in range(4)]
        hT = sb(st, "hT", [128, 16, 512], BF16)
        B_hT = [Buf(f"hT{i}") for i in range(16)]
        pT = [ps(st, f"pT{i}", [128, 2, 512], BF16) for i in range(2)]
        B_pT = [Buf(f"pT{i}") for i in range(2)]
        NPO = 4
        po = [ps(st, f"po{i}", [128, 512], F32) for i in range(NPO)]
        B_po = [Buf(f"po{i}") for i in range(NPO)]
        pa = [ps(st, f"pa{i}", [128, 512], F32) for i in range(2)]
        B_pa = [Buf(f"pa{i}") for i in range(2)]
        cs = sb(st, "cs", [128, 2, 512], F32)
        B_cs = Buf("cs")
        B_sn = Buf("sn")
        NST = 2
        stg = [sb(st, f"stg{i}", [128, 512], F32) for i in range(NST)]
        B_stg = [Buf(f"stg{i}") for i in range(NST)]
        sink = [Buf(f"sink{i}") for i in range(NST)]
        stb = [sb(st, f"stb{i}", [128, 512], BF16) for i in range(NST)]
        B_stb = [Buf(f"stb{i}") for i in range(NST)]
        sinkb = [Buf(f"sinkb{i}") for i in range(NST)]
        sq = sb(st, "sq", [128, 512], BF16)
        B_sq = Buf("sq")
        sd = sb(st, "sd", [128, 512], F32)
        B_sd2 = Buf("sd2")
        rinv = sb(st, "rinv", [128, 512], F32)
        B_rinv = Buf("rinv")
        qn = sb(st, "qn", [128, 512], BF16)
        B_qn = Buf("qn")
        t1 = sb(st, "t1", [128, 512], F32)
        B_t1 = Buf("t1")
        t2 = sb(st, "t2", [128, 512], F32)
        B_t2 = Buf("t2")
        zt = sb(st, "zt", [128, 8, 2], F32)
        B_zt = Buf("zt")
        S.op("pool", lambda e: e.memset(zt[:], 0.0), writes=[B_zt])
        for (nm, n) in (("XRc", CTX), ("XRl", SEQ)):
            xr = d[nm].rearrange("n p t -> p n t")
            S.dma("sp", xr[:, :, 0:2], zt[:], reads=[B_zt], writes=[Buf("h0")])
            S.dma("sp", xr[:, :, 2 + n:4 + n], zt[:], reads=[B_zt], writes=[Buf("h1")])

        cnt = {"x": 0, "po": 0, "pa": 0, "st": 0, "sb": 0, "ev": 0}
        tile_src = []
        for i in range(2):
            tile_src.append(d["ctx_l"][i * 128:(i + 1) * 128, :])
        for i in range(32):
            tile_src.append(d["x_all"][i * 128:(i + 1) * 128, :])
        blocks = [(0, 2, 1, False)] + [(2 + 4 * b, 4, 0, b < 4) for b in range(8)]
        blocks = blocks[:getattr(k, "nblk", 9)]
        tokcol = 0
        for (t0, nt, j, own) in blocks:
            Nt = nt * 128
            for ti in range(nt):
                r = cnt["x"] % NX
                q = cnt["x"] % 4
                cnt["x"] += 1
                S.dma("pool", xt[r][:], tile_src[t0 + ti], writes=[B_xt[r]])
                S.op("act", lambda e, r=r, q=q: e.activation(junk[:], xt[r][:], AF.Square,
                                                             accum_out=stat[:, 0, q:q + 1]),
                     reads=[B_xt[r]], writes=[B_junk, B_ss[q]])
                S.op("act", lambda e, q=q: e.activation(stat[:, 1, q:q + 1], stat[:, 0, q:q + 1], AF.Sqrt,
                                                        bias=k.eps_t[:], scale=1.0 / D),
                     reads=[B_ss[q], k.B_const], writes=[B_sd[q]])
                S.op("dve", lambda e, q=q: e.reciprocal(stat[:, 2, q:q + 1], stat[:, 1, q:q + 1]),
                     reads=[B_sd[q]], writes=[B_rs[q]])
                S.op("dve", lambda e, r=r, q=q, ti=ti: e.tensor_scalar(
                    xn[:, ti, :], xt[r][:], stat[:, 2, q:q + 1], None, op0=ALU.mult),
                    reads=[B_xt[r], B_rs[q]], writes=[B_xn[ti]])
            if k.stage < 2:
                continue
            for cg in range(8):
                pr = cg % 2
                for cl in range(2):
                    c = cg * 2 + cl
                    for ti in range(nt):
                        S.op("pe", lambda e, pr=pr, cl=cl, ti=ti, c=c: e.transpose(
                            pT[pr][:, cl, ti * 128:(ti + 1) * 128], xn[:, ti, c * 128:(c + 1) * 128],
                            k.ident_b[:]), reads=[B_xn[ti], k.B_const], writes=[B_pT[pr]])
                for cl in range(2):
                    c = cg * 2 + cl
                    if k.sub == 1 or (k.sub == 2 and c % 2 == 1) or (k.sub == 3 and c % 2 == 0):
                        continue
                    if cg % 2 == 0:
                        S.op("act", lambda e, pr=pr, cl=cl, c=c, j=j, Nt=Nt: e.activation(
                            hT[:, c, :Nt], pT[pr][:, cl, :Nt], AF.Identity,
                            bias=k.mod[:, c, j:j + 1], scale=k.A1[:, c, j:j + 1]),
                            reads=[B_pT[pr], k.B_mod, k.B_A], writes=[B_hT[c]])
                    else:
                        S.op("dve", lambda e, pr=pr, cl=cl, c=c, j=j, Nt=Nt: e.tensor_scalar(
                            hT[:, c, :Nt], pT[pr][:, cl, :Nt], k.A1[:, c, j:j + 1], k.mod[:, c, j:j + 1],
                            op0=ALU.mult, op1=ALU.add),
                            reads=[B_pT[pr], k.B_mod, k.B_A], writes=[B_hT[c]])
            if k.stage < 3:
                continue
            S.dma("pool", cs[:, 0, :Nt], d["cosT"][:, tokcol:tokcol + Nt], writes=[B_cs])
            S.dma("pool", cs[:, 1, :Nt], d["sinT"][:, tokcol:tokcol + Nt], writes=[B_sn])
            outs = [("k", 1024 + 128 * i, i) for i in range(2)] + [("xr", 1536 + 128 * i, i) for i in range(8)]
            if own:
                outs += [("q", 128 * i, i) for i in range(8)] + [("yr", 2560 + 128 * i, i) for i in range(8)]
            for (kind, col, idx) in outs:
                if (k.stage < 4 or k.sub == 4) and kind != "xr":
                    continue
                r = cnt["po"] % NPO
                cnt["po"] += 1
                for kc in range(16):
                    S.op("pe", lambda e, r=r, kc=kc, col=col, Nt=Nt: e.matmul(
                        po[r][:, :Nt], lhsT=win[:, kc, col:col + 128], rhs=hT[:, kc, :Nt],
                        start=(kc == 0), stop=(kc == 15)), reads=[B_win[kc], B_hT[kc]], writes=[B_po[r]])
                if kind == "xr":
                    s = cnt["st"] % NST
                    cnt["st"] += 1
                    S.op("act", lambda e, s=s, r=r, Nt=Nt: e.activation(stg[s][:, :Nt], po[r][:, :Nt], AF.Identity),
                         reads=[B_po[r]], writes=[B_stg[s]])
                    if j == 1:
                        dst = d["XRc"][idx, :, 2:2 + Nt]
                    else:
                        dst = d["XRl"][idx, :, 2 + tokcol - CTX:2 + tokcol - CTX + Nt]
                    S.dma("sp", dst, stg[s][:, :Nt], reads=[B_stg[s]], writes=[sink[s]])
                elif kind == "yr":
                    s = cnt["st"] % NST
                    cnt["st"] += 1
                    S.op("act", lambda e, s=s, r=r, Nt=Nt: e.activation(stg[s][:, :Nt], po[r][:, :Nt],
                                                                        AF.Gelu_apprx_tanh),
                         reads=[B_po[r]], writes=[B_stg[s]])
                    S.dma("sp", d["YGd"][idx, :, tokcol - CTX:tokcol - CTX + Nt], stg[s][:, :Nt],
                          reads=[B_stg[s]], writes=[sink[s]])
                else:
                    gv = k.gq if kind == "q" else k.gk
                    a = cnt["pa"] % 2
                    a2 = (cnt["pa"] + 1) % 2
                    cnt["pa"] += 2
                    S.op("act", lambda e, r=r, Nt=Nt: e.activation(sq[:, :Nt], po[r][:, :Nt], AF.Square),
                         reads=[B_po[r]], writes=[B_sq])
                    S.op("pe", lambda e, a=a, Nt=Nt: e.matmul(pa[a][:, :Nt], lhsT=k.ones_b[:], rhs=sq[:, :Nt],
                                                              start=True, stop=True),
                         reads=[B_sq, k.B_const], writes=[B_pa[a]])
                    S.op("act", lambda e, a=a, Nt=Nt: e.activation(sd[:, :Nt], pa[a][:, :Nt], AF.Sqrt,
                                                                   bias=k.eps_t[:], scale=1.0 / 128),
                         reads=[B_pa[a], k.B_const], writes=[B_sd2])
                    S.op("dve", lambda e, Nt=Nt: e.reciprocal(rinv[:, :Nt], sd[:, :Nt]),
                         reads=[B_sd2], writes=[B_rinv])
                    S.op("dve", lambda e, r=r, Nt=Nt, gv=gv: e.scalar_tensor_tensor(
                        qn[:, :Nt], in0=po[r][:, :Nt], scalar=gv[:, 0:1], in1=rinv[:, :Nt],
                        op0=ALU.mult, op1=ALU.mult), reads=[B_po[r], B_rinv, k.B_g], writes=[B_qn])
                    S.op("pe", lambda e, a2=a2, Nt=Nt: e.matmul(pa[a2][:, :Nt], lhsT=k.perm_b[:], rhs=qn[:, :Nt],
                                                               start=True, stop=True),
                         reads=[B_qn, k.B_const], writes=[B_pa[a2]])
                    S.op("dve", lambda e, Nt=Nt: e.tensor_tensor(t1[:, :Nt], qn[:, :Nt], cs[:, 0, :Nt], ALU.mult),
                         reads=[B_qn, B_cs], writes=[B_t1])
                    S.op("dve", lambda e, a2=a2, Nt=Nt: e.tensor_tensor(t2[:, :Nt], pa[a2][:, :Nt], cs[:, 1, :Nt],
                                                                       ALU.mult),
                         reads=[B_pa[a2], B_sn], writes=[B_t2])
                    s = cnt["sb"] % NST
                    cnt["sb"] += 1
                    S.op("dve", lambda e, s=s, Nt=Nt: e.tensor_tensor(stb[s][:, :Nt], t1[:, :Nt], t2[:, :Nt], ALU.add),
                         reads=[B_t1, B_t2], writes=[B_stb[s]])
                    if kind == "q":
                        dst = d["Qd"][idx, :, tokcol - CTX:tokcol - CTX + Nt]
                    else:
                        dst = d["Kd"][idx, :, tokcol:tokcol + Nt]
                    S.dma("sp", dst, stb[s][:, :Nt], reads=[B_stb[s]], writes=[sinkb[s]])
            for ti in range(nt if k.stage >= 5 else 0):
                r = cnt["po"] % NPO
                cnt["po"] += 1
                for kc in range(16):
                    S.op("pe", lambda e, r=r, kc=kc, ti=ti: e.matmul(
                        po[r][:, 0:256], lhsT=hT[:, kc, ti * 128:(ti + 1) * 128], rhs=win[:, kc, 1280:1536],
                        start=(kc == 0), stop=(kc == 15)), reads=[B_win[kc], B_hT[kc]], writes=[B_po[r]])
                s = cnt["sb"] % NST
                cnt["sb"] += 1
                S.op("act", lambda e, s=s, r=r: e.activation(stb[s][:, 0:256], po[r][:, 0:256], AF.Identity),
                     reads=[B_po[r]], writes=[B_stb[s]])
                S.dma("sp", d["Vd"][tokcol + ti * 128:tokcol + (ti + 1) * 128, :], stb[s][:, 0:256],
                      reads=[B_stb[s]], writes=[sinkb[s]])
            tokcol += Nt
    S.barrier()


def phase_lru(k):
    nc, S, sb, ps = k.nc, k.S, k.sb, k.ps
    d = k.d
    NF = CTX + TOWN
    with ExitStack() as st:
        xr = sb(st, "xr", [128, NTOK + 8], F32)
        B_xr = Buf("xr")
        B_xr2 = Buf("xr2")
        B_mixs = Buf("mixsink")
        u = sb(st, "u", [128, NTOK], F32)
        B_u = Buf("u")
        ub = sb(st, "ub", [128, NTOK], BF16)
        B_ub = Buf("ub")
        rr = sb(st, "rr", [128, NTOK], F32)
        B_rr = Buf("rr")
        ii = sb(st, "ii", [128, NTOK], F32)
        B_ii = Buf("ii")
        aa = sb(st, "aa", [128, NTOK], F32)
        B_aa = Buf("aa")
        mm = sb(st, "mm", [128, NTOK], F32)
        B_mm = Buf("mm")
        hf = sb(st, "hf", [128, NF], F32)
        B_hf = Buf("hf")
        hb = sb(st, "hb", [128, NTOK], F32)
        B_hb = Buf("hb")
        yg = sb(st, "yg", [128, TOWN], F32)
        B_yg = Buf("yg")
        wg = sb(st, "wg", [128, 2, 2, 128], BF16)
        B_wg = Buf("wg")
        pg = [ps(st, f"pg{i}", [128, 512], F32) for i in range(4)]
        B_pg = [Buf(f"pg{i}") for i in range(4)]
        lv = k.lruv
        sg = sb(st, "sg", [128, 2, 8], F32)
        B_sg = Buf("sg")
        S.op("act", lambda e: e.activation(sg[:], lv[:, :, :, 2], AF.Sigmoid), reads=[k.B_g], writes=[B_sg])
        S.op("act", lambda e: e.activation(sg[:], sg[:], AF.Ln), reads=[B_sg], writes=[B_sg])
        S.op("dve", lambda e: e.tensor_scalar(lv[:, :, :, 3], sg[:], 8.0, None, op0=ALU.mult),
             reads=[B_sg], writes=[k.B_lv])
        S.op("dve", lambda e: e.tensor_scalar(lv[:, :, :, 4], sg[:], 16.0, None, op0=ALU.mult),
             reads=[B_sg], writes=[k.B_lv])
        pcnt = 0
        for n in range(8):
            S.dma("pool", xr[:, 0:CTX + 4], d["XRc"][n], writes=[B_xr])
            S.dma("pool", xr[:, CTX + 4:], d["XRl"][n], writes=[B_xr2])
            S.dma("pool", yg[:], d["YGd"][n], writes=[B_yg])
            S.dma("pool", wg[:], d["wgate"][n], writes=[B_wg])
            for (o0, u0, L) in ((0, 0, CTX), (CTX + 4, CTX, SEQ)):
                S.op("dve", lambda e, o0=o0, u0=u0, L=L, n=n: e.tensor_scalar(
                    u[:, u0:u0 + L], xr[:, o0:o0 + L], k.convw[:, n, 0:1], k.convw[:, n, 5:6],
                    op0=ALU.mult, op1=ALU.add), reads=[B_xr, B_xr2, k.B_g], writes=[B_u])
                for tap in range(1, 5):
                    S.op("dve", lambda e, o0=o0, u0=u0, L=L, n=n, tap=tap: e.scalar_tensor_tensor(
                        u[:, u0:u0 + L], in0=xr[:, o0 + tap:o0 + tap + L], scalar=k.convw[:, n, tap:tap + 1],
                        in1=u[:, u0:u0 + L], op0=ALU.mult, op1=ALU.add), reads=[B_xr, B_xr2, B_u, k.B_g], writes=[B_u])
            S.op("act", lambda e: e.activation(ub[:], u[:], AF.Identity), reads=[B_u], writes=[B_ub])
            for dr in range(2):
                N = NF if dr == 0 else NTOK
                blks = [(c0, min(512, N - c0)) for c0 in range(0, N, 512)]
                for gi, dst, B_dst in ((0, rr, B_rr), (1, ii, B_ii)):
                    for (c0, w) in blks:
                        p = pcnt % 4
                        pcnt += 1
                        S.op("pe", lambda e, p=p, c0=c0, w=w, dr=dr, gi=gi: e.matmul(
                            pg[p][:, :w], lhsT=wg[:, dr, gi, :], rhs=ub[:, c0:c0 + w], start=True, stop=True),
                            reads=[B_wg, B_ub], writes=[B_pg[p]])
                        S.op("act", lambda e, p=p, c0=c0, w=w, dr=dr, gi=gi, dst=dst, n=n: e.activation(
                            dst[:, c0:c0 + w], pg[p][:, :w], AF.Sigmoid, bias=lv[:, dr, n, gi:gi + 1]),
                            reads=[B_pg[p], k.B_g], writes=[B_dst])
                S.op("act", lambda e, N=N, dr=dr, n=n: e.activation(aa[:, :N], rr[:, :N], AF.Exp,
                                                                    scale=lv[:, dr, n, 3:4]),
                     reads=[B_rr, k.B_lv], writes=[B_aa])
                S.op("act", lambda e, N=N, dr=dr, n=n: e.activation(mm[:, :N], rr[:, :N], AF.Exp,
                                                                    scale=lv[:, dr, n, 4:5]),
                     reads=[B_rr, k.B_lv], writes=[B_mm])
                S.op("dve", lambda e, N=N: e.tensor_scalar(mm[:, :N], mm[:, :N], -1.0, 1.0,
                                                           op0=ALU.mult, op1=ALU.add),
                     reads=[B_mm], writes=[B_mm])
                S.op("act", lambda e, N=N: e.activation(mm[:, :N], mm[:, :N], AF.Sqrt), reads=[B_mm], writes=[B_mm])
                S.op("dve", lambda e, N=N: e.tensor_tensor(ii[:, :N], ii[:, :N], mm[:, :N], ALU.mult),
                     reads=[B_ii, B_mm], writes=[B_ii])
                S.op("dve", lambda e, N=N: e.tensor_tensor(ii[:, :N], ii[:, :N], u[:, :N], ALU.mult),
                     reads=[B_ii, B_u], writes=[B_ii])
                if dr == 0:
                    S.op("dve", lambda e: e.tensor_tensor_scan(hf[:, :], data0=aa[:, :NF], data1=ii[:, :NF],
                                                               initial=0.0, op0=ALU.mult, op1=ALU.add),
                         reads=[B_aa, B_ii], writes=[B_hf])
                else:
                    S.op("dve", lambda e: e.tensor_tensor_scan(
                        hb[:, 0:CTX][:, ::-1], data0=aa[:, 0:CTX][:, ::-1], data1=ii[:, 0:CTX][:, ::-1],
                        initial=0.0, op0=ALU.mult, op1=ALU.add), reads=[B_aa, B_ii], writes=[B_hb])
                    S.op("dve", lambda e: e.tensor_tensor_scan(
                        hb[:, CTX:NTOK][:, ::-1], data0=aa[:, CTX:NTOK][:, ::-1], data1=ii[:, CTX:NTOK][:, ::-1],
                        initial=hb[:, 0:1], op0=ALU.mult, op1=ALU.add), reads=[B_aa, B_ii, B_hb], writes=[B_hb])
            S.op("dve", lambda e: e.tensor_tensor(hf[:, CTX:NF], hf[:, CTX:NF], hb[:, CTX:NF], ALU.add),
                 reads=[B_hf, B_hb], writes=[B_hf])
            S.op("dve", lambda e: e.tensor_tensor(yg[:], yg[:], hf[:, CTX:NF], ALU.mult),
                 reads=[B_hf, B_yg], writes=[B_yg])
            S.dma("sp", d["MIXd"][8 + n], yg[:], reads=[B_yg], writes=[B_mixs])
    S.barrier()


def phase_attn(k):
    nc, S, sb, ps = k.nc, k.S, k.sb, k.ps
    d = k.d
    NCH = NTOK // 128
    with ExitStack() as st:
        kT = sb(st, "kT", [128, 2, NTOK], BF16)
        B_kT = Buf("kT")
        vv = sb(st, "vv", [128, NCH, 256], BF16)
        B_vv = Buf("vv")
        qT = [sb(st, f"qT{i}", [128, 8, 512], BF16) for i in range(2)]
        B_qT = [Buf(f"qT{i}") for i in range(2)]
        NP = 4
        pt = [sb(st, f"pt{i}", [128, 512], BF16) for i in range(NP)]
        B_pt = [Buf(f"pt{i}") for i in range(NP)]
        NS = 3
        pss = [ps(st, f"pss{i}", [128, 512], F32) for i in range(NS)]
        B_pss = [Buf(f"pss{i}") for i in range(NS)]
        pso = [ps(st, f"pso{i}", [128, 512], F32) for i in range(2)]
        B_pso = [Buf(f"pso{i}") for i in range(2)]
        psl = [ps(st, f"psl{i}", [128, 512], F32) for i in range(2)]
        B_psl = [Buf(f"psl{i}") for i in range(2)]
        rl = sb(st, "rl", [128, 512], F32)
        B_rl = Buf("rl")
        ao = [sb(st, f"ao{i}", [128, 512], F32) for i in range(2)]
        B_ao = [Buf(f"ao{i}") for i in range(2)]
        sink = [Buf(f"asink{i}") for i in range(2)]
        S.dma("pool", kT[:], d["Kd"].rearrange("h p t -> p h t"), writes=[B_kT])
        S.dma("pool", vv[:], d["Vd"].rearrange("(c p) f -> p c f", p=128), writes=[B_vv])
        sc = 128.0 ** -0.5
        it = 0
        scnt = 0
        for qb in range(4):
            S.dma("pool", qT[qb % 2][:], d["Qd"].rearrange("h p t -> p h t")[:, :, qb * 512:(qb + 1) * 512],
                  writes=[B_qT[qb % 2]])
            for h in range(8):
                kvh = h // 4
                o = it % 2
                it += 1

                def smm(c, h=h, kvh=kvh, qb=qb):
                    nonlocal scnt
                    s = scnt % NS
                    scnt += 1
                    S.op("pe", lambda e, s=s, c=c: e.matmul(
                        pss[s][:], lhsT=kT[:, kvh, c * 128:(c + 1) * 128], rhs=qT[qb % 2][:, h, :],
                        start=True, stop=True), reads=[B_kT, B_qT[qb % 2]], writes=[B_pss[s]])
                    return s
                s_next = smm(0)
                for c in range(NCH):
                    s_cur = s_next
                    if c + 1 < NCH:
                        s_next = smm(c + 1)
                    p = (it * NCH + c) % NP
                    S.op("act", lambda e, p=p, s=s_cur: e.activation(pt[p][:], pss[s][:], AF.Exp, scale=sc),
                         reads=[B_pss[s_cur]], writes=[B_pt[p]])
                    S.op("pe", lambda e, p=p, c=c, o=o, kvh=kvh: e.matmul(
                        pso[o][:], lhsT=vv[:, c, kvh * 128:(kvh + 1) * 128], rhs=pt[p][:],
                        start=(c == 0), stop=(c == NCH - 1)), reads=[B_vv, B_pt[p]], writes=[B_pso[o]])
                    S.op("pe", lambda e, p=p, c=c, o=o: e.matmul(
                        psl[o][:], lhsT=k.ones_b[:], rhs=pt[p][:], start=(c == 0), stop=(c == NCH - 1)),
                        reads=[k.B_const, B_pt[p]], writes=[B_psl[o]])
                S.op("dve", lambda e, o=o: e.reciprocal(rl[:], psl[o][:]), reads=[B_psl[o]], writes=[B_rl])
                S.op("dve", lambda e, o=o: e.tensor_tensor(ao[o][:], pso[o][:], rl[:], ALU.mult),
                     reads=[B_pso[o], B_rl], writes=[B_ao[o]])
                S.dma("sp", d["MIXd"][h, :, qb * 512:(qb + 1) * 512], ao[o][:], reads=[B_ao[o]], writes=[sink[o]])
    S.barrier()


def phase_out(k):
    nc, S, sb, ps = k.nc, k.S, k.sb, k.ps
    d = k.d
    with ExitStack() as st:
        wo = sb(st, "wo", [128, 16, D], BF16)
        B_wo = [Buf(f"wo{i}") for i in range(16)]
        for c in range(16):
            S.dma("pool", wo[:, c, :], d["w_out"][c * 128:(c + 1) * 128, :], writes=[B_wo[c]])
        wr = sb(st, "wr", [128, 16, NE], F32)
        B_wr = Buf("wr")
        S.dma("sp", wr[:], d["w_router"].rearrange("(c p) e -> p c e", p=128), writes=[B_wr])
        br = sb(st, "br", [128, NE], F32)
        B_br = Buf("br")
        S.dma("sp", br[:], d["b_router_b"], writes=[B_br])
        gt1 = sb(st, "gt1", [128, D], F32)
        B_gt1 = Buf("gt1")
        S.dma("sp", gt1[:], d["MODd_b"][2], writes=[B_gt1])
        mx = sb(st, "mx", [128, 16, 512], F32)
        B_mx = [Buf(f"mx{i}") for i in range(16)]
        sq = sb(st, "sq2", [128, 512], BF16)
        B_sq = Buf("sq2")
        sd = sb(st, "sdo", [128, 512], F32)
        B_sd = Buf("sdo")
        rinv = sb(st, "rinvo", [128, 2, 512], F32)
        B_rinv = [Buf("rinv0"), Buf("rinv1")]
        mn = sb(st, "mn", [128, 16, 512], BF16)
        B_mn = [Buf(f"mn{i}") for i in range(16)]
        pss = ps(st, "pss", [128, 512], F32)
        B_pss = Buf("pss")
        pw = [ps(st, f"pw{i}", [128, 512], F32) for i in range(3)]
        B_pw = [Buf(f"pw{i}") for i in range(3)]
        ptr = [ps(st, f"ptr{i}", [128, 4, 128], F32) for i in range(2)]
        B_ptr = [Buf(f"ptr{i}") for i in range(2)]
        prt = ps(st, "prt", [128, NE], F32)
        B_prt = Buf("prt")
        pgt = ps(st, "pgt", [NE, 128], F32)
        B_pgt = Buf("pgt")
        xo = sb(st, "xo", [128, D], F32)
        B_xo = Buf("xo")
        x1 = sb(st, "x1", [128, D], F32)
        B_x1 = Buf("x1")
        junk = sb(st, "junk2", [128, D], BF16)
        B_junk = Buf("junk2")
        stat = sb(st, "stat2", [128, 8], F32)
        B_st = [Buf(f"st2{i}") for i in range(8)]
        xn2 = sb(st, "xn2", [128, D], F32)
        B_xn2 = Buf("xn2")
        h2f = sb(st, "h2f", [128, 16, 128], F32)
        B_h2f = Buf("h2f")
        h2b = sb(st, "h2b", [128, 16, 128], BF16)
        B_h2b = Buf("h2b")
        lg = sb(st, "lg", [128, NE], F32)
        B_lg = Buf("lg")
        t8 = sb(st, "t8", [128, 8], F32)
        B_t8 = Buf("t8")
        msk = sb(st, "msk", [128, NE], F32)
        B_msk = Buf("msk")
        ex = sb(st, "ex", [128, NE], F32)
        B_ex = Buf("ex")
        gg = sb(st, "gg", [128, NE], F32)
        B_gg = Buf("gg")
        gts = sb(st, "gts", [NE, 128], F32)
        B_gts = Buf("gts")
        B_x1s, B_h2s, B_gs, B_gtsk = Buf("x1sink"), Buf("h2sink"), Buf("gsink"), Buf("gtsink")
        pwc = 0
        for tb in range(4):
            for c in range(16):
                S.dma("pool", mx[:, c, :], d["MIXd"][c, :, tb * 512:(tb + 1) * 512], writes=[B_mx[c]])
            for g in range(2):
                for cl in range(8):
                    c = g * 8 + cl
                    S.op("act", lambda e, c=c: e.activation(sq[:], mx[:, c, :], AF.Square),
                         reads=[B_mx[c]], writes=[B_sq])
                    S.op("pe", lambda e, cl=cl: e.matmul(pss[:], lhsT=k.ones_b[:], rhs=sq[:],
                                                         start=(cl == 0), stop=(cl == 7)),
                         reads=[B_sq, k.B_const], writes=[B_pss])
                S.op("act", lambda e: e.activation(sd[:], pss[:], AF.Sqrt, bias=k.eps_t[:], scale=1.0 / 1024),
                     reads=[B_pss, k.B_const], writes=[B_sd])
                S.op("dve", lambda e, g=g: e.reciprocal(rinv[:, g, :], sd[:]), reads=[B_sd], writes=[B_rinv[g]])
                for cl in range(8):
                    c = g * 8 + cl
                    S.op("dve", lambda e, c=c, g=g: e.scalar_tensor_tensor(
                        mn[:, c, :], in0=mx[:, c, :], scalar=k.gmix[:, c:c + 1], in1=rinv[:, g, :],
                        op0=ALU.mult, op1=ALU.mult), reads=[B_mx[c], B_rinv[g], k.B_g], writes=[B_mn[c]])
            for tt in range(4):
                tg = tb * 4 + tt
                S.dma("pool", xo[:], d["x_all"][tg * 128:(tg + 1) * 128, :], writes=[B_xo])
                for dg in range(4):
                    p = pwc % 3
                    pwc += 1
                    for c in range(16):
                        S.op("pe", lambda e, p=p, c=c, tt=tt, dg=dg: e.matmul(
                            pw[p][:], lhsT=mn[:, c, tt * 128:(tt + 1) * 128], rhs=wo[:, c, dg * 512:(dg + 1) * 512],
                            start=(c == 0), stop=(c == 15)), reads=[B_mn[c], B_wo[c]], writes=[B_pw[p]])
                    S.op("dve", lambda e, p=p, dg=dg: e.tensor_tensor(
                        x1[:, dg * 512:(dg + 1) * 512], pw[p][:], gt1[:, dg * 512:(dg + 1) * 512], ALU.mult),
                        reads=[B_pw[p], B_gt1], writes=[B_x1])
                S.op("dve", lambda e: e.tensor_tensor(x1[:], x1[:], xo[:], ALU.add), reads=[B_x1, B_xo], writes=[B_x1])
                S.dma("sp", d["X1d"][tg * 128:(tg + 1) * 128, :], x1[:], reads=[B_x1], writes=[B_x1s])
                q = tg % 2
                S.op("act", lambda e, q=q: e.activation(junk[:], x1[:], AF.Square, accum_out=stat[:, q:q + 1]),
                     reads=[B_x1], writes=[B_junk, B_st[q]])
                S.op("act", lambda e, q=q: e.activation(stat[:, 2 + q:3 + q], stat[:, q:q + 1], AF.Sqrt,
                                                        bias=k.eps_t[:], scale=1.0 / D),
                     reads=[B_st[q], k.B_const], writes=[B_st[2 + q]])
                S.op("dve", lambda e, q=q: e.reciprocal(stat[:, 4 + q:5 + q], stat[:, 2 + q:3 + q]),
                     reads=[B_st[2 + q]], writes=[B_st[4 + q]])
                S.op("dve", lambda e, q=q: e.tensor_scalar(xn2[:], x1[:], stat[:, 4 + q:5 + q], None, op0=ALU.mult),
                     reads=[B_x1, B_st[4 + q]], writes=[B_xn2])
                for cg in range(4):
                    pr = cg % 2
                    for cl in range(4):
                        c = cg * 4 + cl
                        S.op("pe", lambda e, pr=pr, cl=cl, c=c: e.transpose(
                            ptr[pr][:, cl, :], xn2[:, c * 128:(c + 1) * 128], k.ident_f[:]),
                            reads=[B_xn2, k.B_const], writes=[B_ptr[pr]])
                    for cl in range(4):
                        c = cg * 4 + cl
                        S.op("act", lambda e, pr=pr, cl=cl, c=c: e.activation(
                            h2f[:, c, :], ptr[pr][:, cl, :], AF.Identity, bias=k.mod[:, 48 + c, 0:1],
                            scale=k.A2[:, c:c + 1]), reads=[B_ptr[pr], k.B_mod, k.B_A], writes=[B_h2f])
                S.op("dve", lambda e: e.tensor_copy(h2b[:], h2f[:]), reads=[B_h2f], writes=[B_h2b])
                S.dma("sp", d["H2Td"].rearrange("c p t -> p c t")[:, :, tg * 128:(tg + 1) * 128], h2b[:],
                      reads=[B_h2b], writes=[B_h2s])
                for c in range(16):
                    S.op("pe", lambda e, c=c: e.matmul(prt[:], lhsT=h2f[:, c, :], rhs=wr[:, c, :],
                                                       start=(c == 0), stop=(c == 15)),
                         reads=[B_h2f, B_wr], writes=[B_prt])
                S.op("dve", lambda e: e.tensor_tensor(lg[:], prt[:], br[:], ALU.add), reads=[B_prt, B_br], writes=[B_lg])
                S.op("dve", lambda e: e.max(out=t8[:], in_=lg[:]), reads=[B_lg], writes=[B_t8])
                S.op("dve", lambda e: e.tensor_scalar(msk[:], lg[:], t8[:, 3:4], None, op0=ALU.is_ge),
                     reads=[B_lg, B_t8], writes=[B_msk])
                S.op("dve", lambda e: e.tensor_scalar(t8[:, 7:8], t8[:, 0:1], -1.0, None, op0=ALU.mult),
                     reads=[B_t8], writes=[B_t8])
                S.op("act", lambda e: e.activation(ex[:], lg[:], AF.Exp, bias=t8[:, 7:8]),
                     reads=[B_lg, B_t8], writes=[B_ex])
                S.op("dve", lambda e: e.tensor_tensor(ex[:], ex[:], msk[:], ALU.mult), reads=[B_ex, B_msk], writes=[B_ex])
                S.op("dve", lambda e: e.reduce_sum(t8[:, 6:7], ex[:], axis=mybir.AxisListType.X),
                     reads=[B_ex], writes=[B_t8])
                S.op("dve", lambda e: e.reciprocal(t8[:, 5:6], t8[:, 6:7]), reads=[B_t8], writes=[B_t8])
                S.op("dve", lambda e: e.tensor_scalar(gg[:], ex[:], t8[:, 5:6], None, op0=ALU.mult),
                     reads=[B_ex, B_t8], writes=[B_gg])
                S.dma("sp", d["Gd"][tg * 128:(tg + 1) * 128, :], gg[:], reads=[B_gg], writes=[B_gs])
                S.op("pe", lambda e: e.transpose(pgt[:], gg[:], k.ident_f[:]), reads=[B_gg, k.B_const], writes=[B_pgt])
                S.op("act", lambda e: e.activation(gts[:], pgt[:], AF.Identity), reads=[B_pgt], writes=[B_gts])
                S.dma("sp", d["GTd"][:, tg * 128:(tg + 1) * 128], gts[:], reads=[B_gts], writes=[B_gtsk])
    S.barrier()


def phase_moe(k, n_exp=NE):
    nc, S, sb, ps = k.nc, k.S, k.sb, k.ps
    d = k.d
    TH = 1024
    with ExitStack() as st:
        acc = sb(st, "acc", [128, 8, D], F32)
        B_acc = [Buf(f"acc{i}") for i in range(8)]
        h2 = sb(st, "h2", [128, 16, TH], BF16)
        B_h2 = Buf("h2")
        aT = sb(st, "aT", [128, 16, TH], BF16)
        B_aT = [Buf(f"aT{i}") for i in range(16)]
        NG = 2
        wgu = [sb(st, f"wgu{i}", [128, 16, 256], BF16) for i in range(NG)]
        B_wgu = [Buf(f"wgu{i}") for i in range(NG)]
        wdn = [sb(st, f"wdn{i}", [128, 16, 256], BF16) for i in range(NG)]
        B_wdn = [Buf(f"wdn{i}") for i in range(NG)]
        G = sb(st, "G", [128, 16, NE], F32)
        B_G = Buf("G")
        GT = sb(st, "GT", [NE, TOWN], F32)
        B_GT = Buf("GT")
        bdn = sb(st, "bdn", [NE, D], F32)
        B_bdn = Buf("bdn")
        bgu = sb(st, "bgu", [128, NE, 16, 2], F32)
        B_bgu = Buf("bgu")
        NT = 2
        tg_ = [sb(st, f"tg{i}", [128, 512], F32) for i in range(NT)]
        ts_ = [sb(st, f"ts{i}", [128, 512], F32) for i in range(NT)]
        tu_ = [sb(st, f"tu{i}", [128, 512], F32) for i in range(NT)]
        B_tg = [Buf(f"tg{i}") for i in range(NT)]
        B_ts = [Buf(f"ts{i}") for i in range(NT)]
        B_tu = [Buf(f"tu{i}") for i in range(NT)]
        pgu = [ps(st, f"pgu{i}", [128, 512], F32) for i in range(4)]
        B_pgu = [Buf(f"pgu{i}") for i in range(4)]
        py = [ps(st, f"py{i}", [128, 512], F32) for i in range(4)]
        B_py = [Buf(f"py{i}") for i in range(4)]
        S.dma("sp", G[:], d["Gd"].rearrange("(t p) e -> p t e", p=128), writes=[B_G])
        S.dma("sp", GT[:], d["GTd"], writes=[B_GT])
        S.dma("sp", bdn[:], d["b_down"], writes=[B_bdn])
        S.dma("sp", bgu[:], d["bgu_l"], writes=[B_bgu])
        cp = 0
        cy = 0
        ct = 0
        jobs = []
        for hf in range(2):
            for ex in range(n_exp):
                jobs += [("gu", hf, ex, jc) for jc in range(16)]
                jobs += [("dn", hf, ex, dg) for dg in range(8)]
        ring = {"gu": 0, "dn": 0}
        slot_of = {}

        def issue(i):
            kind, hf_, ex_, idx = jobs[i]
            w = ring[kind] % NG
            ring[kind] += 1
            slot_of[i] = w
            if kind == "gu":
                src = d["w_gate_up"][ex_].rearrange("(kc p) f -> p kc f", p=128)[:, :, idx * 256:(idx + 1) * 256]
                S.dma("pool", wgu[w][:], src, writes=[B_wgu[w]])
            else:
                src = d["w_down"][ex_].rearrange("(kc p) f -> p kc f", p=128)[:, :, idx * 256:(idx + 1) * 256]
                S.dma("pool", wdn[w][:], src, writes=[B_wdn[w]])
        issue(0)
        ji = 0
        for hf in range(2):
            S.dma("sp", h2[:], d["H2Td"].rearrange("c p t -> p c t")[:, :, hf * TH:(hf + 1) * TH], writes=[B_h2])
            for tt in range(8):
                tgl = hf * 8 + tt
                for dg in range(4):
                    p = cy % 4
                    cy += 1
                    S.op("pe", lambda e, p=p, tgl=tgl, dg=dg: e.matmul(
                        py[p][:], lhsT=GT[:, tgl * 128:(tgl + 1) * 128], rhs=bdn[:, dg * 512:(dg + 1) * 512],
                        start=True, stop=True), reads=[B_GT, B_bdn], writes=[B_py[p]])
                    S.op("act", lambda e, p=p, tt=tt, dg=dg: e.activation(acc[:, tt, dg * 512:(dg + 1) * 512], py[p][:], AF.Identity),
                         reads=[B_py[p]], writes=[B_acc[tt]])
            for ex in range(n_exp):
                for jc in range(16):
                    w = slot_of[ji]
                    if ji + 1 < len(jobs):
                        issue(ji + 1)
                    ji += 1
                    for tb in range(2):
                        pg_, pu_ = cp % 4, (cp + 1) % 4
                        cp += 2
                        for (pp, off) in ((pg_, 0), (pu_, 1)):
                            for kc in range(16):
                                S.op("pe", lambda e, pp=pp, off=off, kc=kc, w=w, tb=tb: e.matmul(
                                    pgu[pp][:], lhsT=wgu[w][:, kc, off::2], rhs=h2[:, kc, tb * 512:(tb + 1) * 512],
                                    start=(kc == 0), stop=(kc == 15)), reads=[B_wgu[w], B_h2], writes=[B_pgu[pp]])
                        t = ct % NT
                        ct += 1
                        S.op("dve", lambda e, t=t, pg_=pg_, ex=ex, jc=jc: e.tensor_scalar(
                            tg_[t][:], pgu[pg_][:], bgu[:, ex, jc, 0:1], 7.0, op0=ALU.add, op1=ALU.min),
                            reads=[B_pgu[pg_], B_bgu], writes=[B_tg[t]])
                        S.op("act", lambda e, t=t: e.activation(ts_[t][:], tg_[t][:], AF.Sigmoid, scale=1.702),
                             reads=[B_tg[t]], writes=[B_ts[t]])
                        S.op("dve", lambda e, t=t, pu_=pu_, ex=ex, jc=jc: e.tensor_scalar(
                            tu_[t][:], pgu[pu_][:], bgu[:, ex, jc, 1:2], -7.0, op0=ALU.add, op1=ALU.max),
                            reads=[B_pgu[pu_], B_bgu], writes=[B_tu[t]])
                        S.op("dve", lambda e, t=t: e.tensor_scalar(tu_[t][:], tu_[t][:], 7.0, 1.0,
                                                                   op0=ALU.min, op1=ALU.add),
                             reads=[B_tu[t]], writes=[B_tu[t]])
                        S.op("dve", lambda e, t=t: e.tensor_tensor(tg_[t][:], tg_[t][:], ts_[t][:], ALU.mult),
                             reads=[B_tg[t], B_ts[t]], writes=[B_tg[t]])
                        S.op("dve", lambda e, t=t, jc=jc, tb=tb: e.tensor_tensor(
                            aT[:, jc, tb * 512:(tb + 1) * 512], tg_[t][:], tu_[t][:], ALU.mult),
                            reads=[B_tg[t], B_tu[t]], writes=[B_aT[jc]])
                for dg in range(8):
                    w = slot_of[ji]
                    if ji + 1 < len(jobs):
                        issue(ji + 1)
                    ji += 1
                    for tt in range(8):
                        p = cy % 4
                        cy += 1
                        tgl = hf * 8 + tt
                        for jc in range(16):
                            S.op("pe", lambda e, p=p, jc=jc, tt=tt, w=w: e.matmul(
                                py[p][:, 0:256], lhsT=aT[:, jc, tt * 128:(tt + 1) * 128], rhs=wdn[w][:, jc, :],
                                start=(jc == 0), stop=(jc == 15)), reads=[B_aT[jc], B_wdn[w]], writes=[B_py[p]])
                        S.op("dve", lambda e, p=p, tt=tt, dg=dg, tgl=tgl, ex=ex: e.scalar_tensor_tensor(
                            acc[:, tt, dg * 256:(dg + 1) * 256], in0=py[p][:, 0:256], scalar=G[:, tgl, ex:ex + 1],
                            in1=acc[:, tt, dg * 256:(dg + 1) * 256], op0=ALU.mult, op1=ALU.add),
                            reads=[B_py[p], B_G, B_acc[tt]], writes=[B_acc[tt]])
            for tt in range(8):
                tgl = hf * 8 + tt
                for dg in range(4):
                    t = ct % NT
                    ct += 1
                    sl = slice(dg * 512, (dg + 1) * 512)
                    S.dma("pool", tg_[t][:], d["X1d"][tgl * 128:(tgl + 1) * 128, sl], writes=[B_tg[t]])
                    S.dma("pool", ts_[t][:], d["MODd_b"][5][:, sl], writes=[B_ts[t]])
                    S.op("dve", lambda e, t=t, tt=tt, sl=sl: e.tensor_tensor(tu_[t][:], acc[:, tt, sl], ts_[t][:], ALU.mult),
                         reads=[B_acc[tt], B_ts[t]], writes=[B_tu[t]])
                    S.op("dve", lambda e, t=t: e.tensor_tensor(tu_[t][:], tu_[t][:], tg_[t][:], ALU.add),
                         reads=[B_tu[t], B_tg[t]], writes=[B_tu[t]])
                    if len(k.out_bufs) < 4:
                        k.out_bufs.append(Buf(f"outsink{len(k.out_bufs)}"))
                    ob = k.out_bufs[(tgl * 4 + dg) % 4]
                    S.dma("sp", d["out"][tgl * 128:(tgl + 1) * 128, sl], tu_[t][:], reads=[B_tu[t]], writes=[ob])
    S.op("sp", lambda e: e.nop(), reads=k.out_bufs)


IN_SPECS = {
    "x_all": ([SEQ, D], F32), "ctx_l": ([CTX, D], F32), "cT": ([128, 16, 2], F32),
    "w_mod": ([D, 6 * D], F32), "bm": ([128, 96], F32), "gvec": ([128, 16, 4], F32),
    "w_in": ([D, 3584], F32), "gqk": ([128, 2], F32), "convw": ([128, 8, 6], F32),
    "wgate": ([8, 128, 2, 2, 128], F32), "lruv": ([128, 2, 8, 5], F32),
    "w_out": ([D, D], F32), "w_router": ([D, NE], F32), "b_router_b": ([128, NE], F32),
    "w_gate_up": ([NE, D, 2 * D], F32), "bgu_l": ([128, NE, 16, 2], F32),
    "w_down": ([NE, D, D], F32), "b_down": ([NE, D], F32),
    "cosT": ([128, NTOK], F32), "sinT": ([128, NTOK], F32),
    "ident_f": ([128, 128], F32), "cb": ([128, 3, 128], BF16),
}
SCRATCH = {
    "MODd": ([96, 128], F32), "XRc": ([8, 128, CTX + 4], F32), "XRl": ([8, 128, SEQ + 4], F32),
    "YGd": ([8, 128, TOWN], F32), "Qd": ([8, 128, TOWN], BF16), "Kd": ([2, 128, NTOK], BF16),
    "Vd": ([NTOK, 256], BF16), "MIXd": ([16, 128, TOWN], F32), "X1d": ([TOWN, D], F32),
    "H2Td": ([16, 128, TOWN], BF16), "Gd": ([TOWN, NE], F32), "GTd": ([NE, TOWN], F32),
}


def build(phases="0ALBOM", dbg_in=(), dbg_out=(), n_exp=NE, skip=(), nblk=9, stage=9, sub=0):
    nc = bass.Bass("TRN2", target_bir_lowering=False)
    k = K()
    k.nblk = nblk
    k.stage = stage
    k.sub = sub
    k.nc = nc
    k.S = Sched(nc)
    _mk(k)
    d = {}
    for n, (shp, dt) in IN_SPECS.items():
        if n in skip:
            continue
        d[n] = nc.dram_tensor(n, shp, dt, kind="ExternalInput").ap()
    for n, (shp, dt) in SCRATCH.items():
        kind = "ExternalInput" if n in dbg_in else ("ExternalOutput" if n in dbg_out else "Internal")
        d[n] = nc.dram_tensor(n, shp, dt, kind=kind).ap()
    d["out"] = nc.dram_tensor("out", [TOWN, D], F32, kind="ExternalOutput").ap()
    md = d["MODd"]
    d["MODd_b"] = [md[s * 16:(s + 1) * 16, :].rearrange("c f -> (c f)").partition_broadcast(128) for s in range(6)]
    k.d = d
    k.out_bufs = []
    S = k.S
    with ExitStack() as st:
        sb = k.sb
        k.ident_f = sb(st, "ident_f", [128, 128], F32)
        cb = sb(st, "cb", [128, 3, 128], BF16)
        k.ident_b, k.ones_b, k.perm_b = cb[:, 0, :], cb[:, 1, :], cb[:, 2, :]
        k.eps_t = sb(st, "eps_t", [128, 1], F32)
        k.B_const = Buf("const")
        k.mod = sb(st, "mod", [128, 96, 2], F32)
        k.B_mod = Buf("mod")
        k.A1 = sb(st, "A1", [128, 16, 2], F32)
        k.A2 = sb(st, "A2", [128, 16], F32)
        k.B_A = Buf("A")
        gvec = sb(st, "gvec", [128, 16, 4], F32)
        k.g1, k.g2, k.gmix = gvec[:, :, 0], gvec[:, :, 1], gvec[:, :, 2]
        gqk = sb(st, "gqk", [128, 2], F32)
        k.gq, k.gk = gqk[:, 0:1], gqk[:, 1:2]
        k.convw = sb(st, "convw", [128, 8, 6], F32)
        k.lruv = sb(st, "lruv", [128, 2, 8, 5], F32)
        k.B_g = Buf("g")
        k.B_lv = Buf("lv")
        S.dma("sp", k.ident_f[:], d["ident_f"], writes=[k.B_const])
        S.dma("sp", cb[:], d["cb"], writes=[Buf("cb")])
        S.op("dve", lambda e: e.memset(k.eps_t[:], EPS), writes=[Buf("epsb")])
        S.dma("sp", gvec[:], d["gvec"], writes=[k.B_g])
        S.dma("sp", gqk[:], d["gqk"], writes=[Buf("gqk")])
        S.dma("sp", k.convw[:], d["convw"], writes=[Buf("cw")])
        S.dma("sp", k.lruv[:], d["lruv"], writes=[k.B_lv])
        S.barrier()
        if "0" in phases:
            phase_mod(k)
        if "A" in phases:
            phase_proj(k)
        if "L" in phases:
            phase_lru(k)
        if "B" in phases:
            phase_attn(k)
        if "O" in phases:
            phase_out(k)
        if "M" in phases:
            phase_moe(k, n_exp)
        S.emit()
    return nc


def _rope_tables(half):
    GRID_W = 64
    pos = np.arange(SEQ)
    if half == 1:
        pos = pos[::-1]
    row = (pos // GRID_W).astype(np.float32)
    col = (pos % GRID_W).astype(np.float32)
    inv = (10000.0 ** (-np.arange(32, dtype=np.float32) / 32)).astype(np.float32)
    cosT = np.ones((128, NTOK), np.float32)
    sinT = np.zeros((128, NTOK), np.float32)
    for dd in range(128):
        ax = dd // 64
        hh = (dd % 64) // 32
        f = dd % 32
        ang = (row if ax == 0 else col) * inv[f]
        cosT[dd, CTX:] = np.cos(ang)
        sinT[dd, CTX:] = np.sin(ang) * (-1.0 if hh == 0 else 1.0)
    return cosT, sinT


def _consts():
    ident = np.eye(128, dtype=np.float32)
    perm = np.zeros((128, 128), np.float32)
    for dd in range(128):
        p = dd + 32 if (dd % 64) < 32 else dd - 32
        perm[p, dd] = 1.0
    cb = np.stack([ident, np.ones((128, 128), np.float32), perm], axis=1).astype(ml_dtypes.bfloat16)
    return ident, cb


def make_in_maps(inp):
    f = lambda a: np.ascontiguousarray(np.asarray(a, dtype=np.float32))
    x, c, ctx, c_ctx = f(inp["x"]), f(inp["c"]), f(inp["ctx"]), f(inp["c_ctx"])
    w_mod, b_mod = f(inp["w_mod"])[0], f(inp["b_mod"])[0]
    w_in = f(inp["w_in"])[0]
    w_out = f(inp["w_out"])[0]
    w_router = f(inp["w_router"])[0]
    b_router = f(inp["b_router"])[0]
    w_gu, b_gu = f(inp["w_gate_up"])[0], f(inp["b_gate_up"])[0]
    w_dn, b_dn = f(inp["w_down"])[0], f(inp["b_down"])[0]
    ident, cb = _consts()
    chunk = lambda v: np.ascontiguousarray(v.reshape(-1, 128).T)
    gvec = np.zeros((128, 16, 4), np.float32)
    gvec[:, :, 0] = chunk(f(inp["g_norm1"])[0])
    gvec[:, :, 1] = chunk(f(inp["g_norm2"])[0])
    gvec[:, :, 2] = chunk(np.concatenate([f(inp["g_att_out"])[0], f(inp["g_rec_out"])[0]]))
    gqk = np.stack([f(inp["g_q"])[0], f(inp["g_k"])[0]], axis=1)
    bm = chunk(b_mod)
    conv_w, conv_b = f(inp["conv_w"])[0], f(inp["conv_b"])[0]
    wa, ba = f(inp["w_gate_a"])[0], f(inp["b_gate_a"])[0]
    wx, bx = f(inp["w_gate_x"])[0], f(inp["b_gate_x"])[0]
    lam = f(inp["lru_lambda"])[0]
    bgu_l = np.ascontiguousarray(b_gu.reshape(NE, 16, 128, 2).transpose(2, 0, 1, 3))
    b_router_b = np.ascontiguousarray(np.broadcast_to(b_router[None, :], (128, NE)))
    maps = []
    for core in range(NCORES):
        b, half = core // 2, core % 2
        xa = x[b] if half == 0 else x[b][::-1]
        cl = ctx[b] if half == 0 else ctx[b][::-1]
        cT = np.stack([chunk(c[b]), chunk(c_ctx)], axis=2)
        dirs = (0, 1) if half == 0 else (1, 0)
        convw = np.zeros((128, 8, 6), np.float32)
        taps = [0, 1, 2, 3] if half == 0 else None
        for n in range(8):
            sl = slice(n * 128, (n + 1) * 128)
            if half == 0:
                convw[:, n, 0:4] = conv_w[:, sl].T
            else:
                convw[:, n, 1:5] = conv_w[::-1, sl].T
            convw[:, n, 5] = conv_b[sl]
        wgate = np.zeros((8, 128, 2, 2, 128), np.float32)
        lruv = np.zeros((128, 2, 8, 5), np.float32)
        for li, dr in enumerate(dirs):
            for n in range(8):
                sl = slice(n * 128, (n + 1) * 128)
                wgate[n, :, li, 0, :] = wa[dr, n]
                wgate[n, :, li, 1, :] = wx[dr, n]
                lruv[:, li, n, 0] = ba[dr, sl]
                lruv[:, li, n, 1] = bx[dr, sl]
                lruv[:, li, n, 2] = lam[dr, sl]
        cosT, sinT = _rope_tables(half)
        maps.append({
            "x_all": np.ascontiguousarray(xa), "ctx_l": np.ascontiguousarray(cl), "cT": np.ascontiguousarray(cT),
            "w_mod": w_mod, "bm": bm, "gvec": gvec, "w_in": w_in, "gqk": np.ascontiguousarray(gqk),
            "convw": convw, "wgate": wgate, "lruv": lruv, "w_out": w_out, "w_router": w_router,
            "b_router_b": b_router_b, "w_gate_up": w_gu, "bgu_l": bgu_l, "w_down": w_dn, "b_down": b_dn,
            "cosT": cosT, "sinT": sinT, "ident_f": ident, "cb": cb,
        })
    return maps


_NC = None


def kernel(**inputs):
    global _NC
    if _NC is None:
        _NC = build()
    maps = make_in_maps(inputs)
    res = run_bass_kernel_spmd(_NC, maps, core_ids=list(range(NCORES)))
    out = np.zeros((4, SEQ, D), np.float32)
    for core in range(NCORES):
        b, half = core // 2, core % 2
        o = res.results[core]["out"]
        if half == 0:
            out[b, :TOWN] = o
        else:
            out[b, TOWN:] = o[::-1]
    return out
```

```python
import numpy as np
import ml_dtypes
from contextlib import ExitStack
import concourse.bass as bass
import concourse.mybir as mybir
from concourse.bass_utils import run_bass_kernel_spmd

F32 = mybir.dt.float32
BF16 = mybir.dt.bfloat16
AF = mybir.ActivationFunctionType
ALU = mybir.AluOpType

D = 2048
SEQ = 4096
TOWN = 2048
CTX = 256
NTOK = CTX + SEQ
NE = 32
EPS = 1e-6
NCORES = 8

SEM_ROT = 30000


class Buf:
    __slots__ = ("name", "last_w", "readers", "dsem", "dcount")

    def __init__(self, name):
        self.name = name
        self.last_w = None
        self.readers = []
        self.dsem = None
        self.dcount = 0


class Op:
    __slots__ = ("eng", "fn", "deps", "dma", "dbuf", "need", "sem", "val")

    def __init__(self, eng, fn, dma, dbuf):
        self.eng = eng
        self.fn = fn
        self.deps = []
        self.dma = dma
        self.dbuf = dbuf
        self.need = False
        self.sem = None
        self.val = 0


class Sched:
    ENGS = ("pe", "act", "dve", "pool", "sp")

    def __init__(self, nc):
        self.nc = nc
        self.ops = []
        self.last = {e: None for e in self.ENGS}
        self.open_dmas = []

    def op(self, eng, fn, reads=(), writes=()):
        o = Op(eng, fn, False, None)
        self._deps(o, reads, writes)
        self.ops.append(o)
        self.last[eng] = o
        return o

    def dma(self, eng, out, in_, reads=(), writes=(), **kw):
        o = Op(eng, lambda e: e.dma_start(out=out, in_=in_, **kw), True, writes[0])
        self._deps(o, reads, writes)
        self.ops.append(o)
        self.open_dmas.append(o)
        return o

    def _deps(self, o, reads, writes):
        deps = o.deps
        for b in reads:
            w = b.last_w
            if w is not None:
                deps.append(w)
        for b in writes:
            w = b.last_w
            if w is not None and (w.dma or o.dma or w.eng != o.eng):
                deps.append(w)
            for r in b.readers:
                if r.dma or o.dma or r.eng != o.eng:
                    deps.append(r)
        for b in reads:
            b.readers.append(o)
        for b in writes:
            b.last_w = o
            b.readers = []

    def barrier(self):
        lasts = [o for o in self.last.values() if o is not None]
        dmas = self.open_dmas
        self.open_dmas = []
        news = []
        for e in self.ENGS:
            o = Op(e, lambda en: en.nop(), False, None)
            o.deps = [x for x in lasts if x.eng != e] + list(dmas)
            news.append(o)
        news[0].dbuf = "BARRIER"
        for o in news:
            self.ops.append(o)
            self.last[o.eng] = o

    def emit(self):
        nc = self.nc
        ops = self.ops
        for o in ops:
            if o.dma:
                o.need = True
            for d in o.deps:
                d.need = True
        stack = ExitStack()
        cur = {e: [None, 0] for e in self.ENGS}
        nsem = [0]

        def new_sem(tag):
            nsem[0] += 1
            return stack.enter_context(nc.semaphore(f"s{nsem[0]}_{tag}"))

        free_d = []
        scount = {}
        active = []
        for o in ops:
            if o.dbuf == "BARRIER":
                for b in active:
                    free_d.append(b.dsem)
                    b.dsem = None
                active = []
            if not o.need:
                continue
            if o.dma:
                b = o.dbuf
                if b.dsem is None:
                    b.dsem = free_d.pop() if free_d else new_sem("d")
                    active.append(b)
                k_ = id(b.dsem)
                scount[k_] = scount.get(k_, 0) + 16
                o.sem, o.val = b.dsem, scount[k_]
            else:
                c = cur[o.eng]
                if c[0] is None or c[1] >= SEM_ROT:
                    c[0] = new_sem(o.eng)
                    c[1] = 0
                c[1] += 1
                o.sem, o.val = c[0], c[1]
        self.nsem = nsem[0]
        streams = {e: [] for e in self.ENGS}
        waited = {e: {} for e in self.ENGS}
        for o in ops:
            wl = {}
            wd = waited[o.eng]
            for d in o.deps:
                k = id(d.sem)
                if wd.get(k, 0) >= d.val:
                    continue
                if k not in wl or wl[k][1] < d.val:
                    wl[k] = (d.sem, d.val)
            for k, (s, v) in wl.items():
                wd[k] = v
            streams[o.eng].append((o, list(wl.values())))
        with nc.Block() as block:
            def mk(ename):
                def body(e):
                    for o, wl in streams[ename]:
                        for s, v in wl:
                            e.wait_ge(s, v)
                        ins = o.fn(e)
                        if o.need:
                            ins.then_inc(o.sem, 16 if o.dma else 1)
                return body
            block.tensor(mk("pe"))
            block.scalar(mk("act"))
            block.vector(mk("dve"))
            block.gpsimd(mk("pool"))
            block.sync(mk("sp"))
        stack.close()


class K:
    pass


def _mk(k):
    nc = k.nc

    def sb(st, name, shape, dt):
        return st.enter_context(nc.sbuf_tensor("s_" + name, shape, dt))

    def ps(st, name, shape, dt):
        return st.enter_context(nc.psum_tensor("p_" + name, shape, dt))
    k.sb, k.ps = sb, ps


def phase_mod(k):
    nc, S, sb, ps = k.nc, k.S, k.sb, k.ps
    d = k.d
    with ExitStack() as st:
        cT = sb(st, "cT", [128, 16, 2], F32)
        scT = sb(st, "scT", [128, 16, 2], F32)
        bmr = sb(st, "bmr", [2, 6 * D], F32)
        mrow = sb(st, "mrow", [2, 6 * D], F32)
        pm2 = [ps(st, f"pm2{i}", [2, 512], F32) for i in range(2)]
        pfm = ps(st, "pfm", [128, 96, 2], F32)
        B_c, B_sc, B_bmr, B_mrow, B_pfm = (Buf(n) for n in "c sc bmr mrow pfm".split())
        B_pm2 = [Buf("pm20"), Buf("pm21")]
        NSL = 2
        slabs = [sb(st, f"wm{i}", [128, 16, 512], F32) for i in range(NSL)]
        B_sl = [Buf(f"wm{i}") for i in range(NSL)]
        S.dma("sp", cT[:], d["cT"], writes=[B_c])
        S.dma("sp", bmr[:], d["bm_row"], writes=[B_bmr])
        S.op("act", lambda e: e.activation(scT[:], cT[:], AF.Silu), reads=[B_c], writes=[B_sc])
        wm = d["w_mod"].rearrange("(kc p) f -> p kc f", p=128)
        for g in range(24):
            sl, B = slabs[g % NSL], B_sl[g % NSL]
            p = g % 2
            S.dma("sp", sl[:], wm[:, :, g * 512:(g + 1) * 512], writes=[B])
            for kc in range(16):
                S.op("pe", lambda e, sl=sl, kc=kc, p=p: e.matmul(
                    pm2[p][:, :], lhsT=scT[:, kc, :], rhs=sl[:, kc, :],
                    start=(kc == 0), stop=(kc == 15)), reads=[B, B_sc], writes=[B_pm2[p]])
            S.op("dve", lambda e, g=g, p=p: e.tensor_tensor(
                mrow[:, g * 512:(g + 1) * 512], pm2[p][:, :], bmr[:, g * 512:(g + 1) * 512], ALU.add),
                reads=[B_pm2[p], B_bmr], writes=[B_mrow])
        mod = k.mod
        for fc in range(96):
            S.op("pe", lambda e, fc=fc: e.matmul(
                pfm[:, fc, :], lhsT=mrow[:, fc * 128:(fc + 1) * 128], rhs=k.ident_f[0:2, 0:2],
                start=True, stop=True), reads=[B_mrow, k.B_const], writes=[B_pfm])
        S.op("dve", lambda e: e.tensor_copy(mod[:], pfm[:]), reads=[B_pfm], writes=[k.B_mod])
        for j in range(2):
            S.op("dve", lambda e, j=j: e.scalar_tensor_tensor(
                k.A1[:, :, j], in0=mod[:, 16:32, j], scalar=1.0, in1=k.g1[:], op0=ALU.add, op1=ALU.mult),
                reads=[k.B_mod, k.B_g], writes=[k.B_A])
        S.op("dve", lambda e: e.scalar_tensor_tensor(
            k.A2[:], in0=mod[:, 64:80, 0], scalar=1.0, in1=k.g2[:], op0=ALU.add, op1=ALU.mult),
            reads=[k.B_mod, k.B_g], writes=[k.B_A])
        S.dma("sp", d["MODd"].rearrange("(o c) f -> o (c f)", o=1), mrow[0:1, :], reads=[B_mrow],
              writes=[Buf("modd")])
    S.barrier()


def phase_proj(k):
    nc, S, sb, ps = k.nc, k.S, k.sb, k.ps
    d = k.d
    with ExitStack() as st:
        win = sb(st, "win", [128, 16, 3584], BF16)
        B_win = [Buf(f"win{i}") for i in range(16)]
        for kc in range(16):
            S.dma("pool", win[:, kc, :], d["w_in"][kc * 128:(kc + 1) * 128, :], writes=[B_win[kc]])
        NX = 2
        xt = [sb(st, f"xt{i}", [128, D], F32) for i in range(NX)]
        B_xt = [Buf(f"xt{i}") for i in range(NX)]
        junk = sb(st, "junk", [128, D], BF16)
        B_junk = Buf("junk")
        stat = sb(st, "stat", [128, 3, 4], F32)
        B_ss = [Buf(f"ss{i}") for i in range(4)]
        B_sd = [Buf(f"sd{i}") for i in range(4)]
        B_rs = [Buf(f"rs{i}") for i in range(4)]
        xn = sb(st, "xn", [128, 4, D], BF16)
        B_xn = [Buf(f"xn{i}") for i in range(4)]
        hT = sb(st, "hT", [128, 16, 512], BF16)
        B_hT = [Buf(f"hT{i}") for i in range(16)]
        pT = [ps(st, f"pT{i}", [128, 2, 512], BF16) for i in range(2)]
        B_pT = [Buf(f"pT{i}") for i in range(2)]
        NPO = 4
        po = [ps(st, f"po{i}", [128, 512], F32) for i in range(NPO)]
        B_po = [Buf(f"po{i}") for i in range(NPO)]
        pa = [ps(st, f"pa{i}", [128, 512], F32) for i in range(2)]
        B_pa = [Buf(f"pa{i}") for i in range(2)]
        cs = sb(st, "cs", [128, 2, 512], F32)
        B_cs = Buf("cs")
        B_sn = Buf("sn")
        NST = 2
        stg = [sb(st, f"stg{i}", [128, 512], F32) for i in range(NST)]
        B_stg = [Buf(f"stg{i}") for i in range(NST)]
        sink = [Buf(f"sink{i}") for i in range(NST)]
        stb = [sb(st, f"stb{i}", [128, 512], BF16) for i in range(NST)]
        B_stb = [Buf(f"stb{i}") for i in range(NST)]
        sinkb = [Buf(f"sinkb{i}") for i in range(NST)]
        sq = sb(st, "sq", [128, 512], BF16)
        B_sq = Buf("sq")
        sd = sb(st, "sd", [128, 512], F32)
        B_sd2 = Buf("sd2")
        rinv = sb(st, "rinv", [128, 512], F32)
        B_rinv = Buf("rinv")
        qn = sb(st, "qn", [128, 512], BF16)
        B_qn = Buf("qn")
        t1 = sb(st, "t1", [128, 512], F32)
        B_t1 = Buf("t1")
        t2 = sb(st, "t2", [128, 512], F32)
        B_t2 = Buf("t2")
        zt = sb(st, "zt", [128, 8, 2], F32)
        B_zt = Buf("zt")
        S.op("pool", lambda e: e.memset(zt[:], 0.0), writes=[B_zt])
        for (nm, n) in (("XRc", CTX), ("XRl", SEQ)):
            xr = d[nm].rearrange("n p t -> p n t")
            S.dma("sp", xr[:, :, 0:2], zt[:], reads=[B_zt], writes=[Buf("h0")])
            S.dma("sp", xr[:, :, 2 + n:4 + n], zt[:], reads=[B_zt], writes=[Buf("h1")])

        cnt = {"x": 0, "po": 0, "pa": 0, "st": 0, "sb": 0, "ev": 0}
        tile_src = []
        for i in range(2):
            tile_src.append(d["ctx_l"][i * 128:(i + 1) * 128, :])
        for i in range(32):
            tile_src.append(d["x_all"][i * 128:(i + 1) * 128, :])
        blocks = [(0, 2, 1, False)] + [(2 + 4 * b, 4, 0, b < 4) for b in range(8)]
        blocks = blocks[:getattr(k, "nblk", 9)]
        tokcol = 0
        n_tiles = sum(b_[1] for b_ in blocks)

        def xload(gi):
            if gi < n_tiles:
                S.dma("sp", xt[gi % NX][:], tile_src[gi], writes=[B_xt[gi % NX]])
        xload(0)
        xload(1)
        for (t0, nt, j, own) in blocks:
            Nt = nt * 128
            for ti in range(nt):
                r = cnt["x"] % NX
                q = cnt["x"] % 4
                cnt["x"] += 1
                S.op("act", lambda e, r=r, q=q: e.activation(junk[:], xt[r][:], AF.Square,
                                                             accum_out=stat[:, 0, q:q + 1]),
                     reads=[B_xt[r]], writes=[B_junk, B_ss[q]])
                S.op("act", lambda e, q=q: e.activation(stat[:, 1, q:q + 1], stat[:, 0, q:q + 1], AF.Sqrt,
                                                        bias=k.eps_t[:], scale=1.0 / D),
                     reads=[B_ss[q], k.B_const], writes=[B_sd[q]])
                S.op("dve", lambda e, q=q: e.reciprocal(stat[:, 2, q:q + 1], stat[:, 1, q:q + 1]),
                     reads=[B_sd[q]], writes=[B_rs[q]])
                S.op("dve", lambda e, r=r, q=q, ti=ti: e.tensor_scalar(
                    xn[:, ti, :], xt[r][:], stat[:, 2, q:q + 1], None, op0=ALU.mult),
                    reads=[B_xt[r], B_rs[q]], writes=[B_xn[ti]])
                xload(t0 + ti + NX)
            if k.stage < 2:
                continue
            for cg in range(8):
                pr = cg % 2
                for cl in range(2):
                    c = cg * 2 + cl
                    for ti in range(nt):
                        S.op("pe", lambda e, pr=pr, cl=cl, ti=ti, c=c: e.transpose(
                            pT[pr][:, cl, ti * 128:(ti + 1) * 128], xn[:, ti, c * 128:(c + 1) * 128],
                            k.ident_b[:]), reads=[B_xn[ti], k.B_const], writes=[B_pT[pr]])
                for cl in range(2):
                    c = cg * 2 + cl
                    if k.sub == 1 or (k.sub == 2 and c % 2 == 1) or (k.sub == 3 and c % 2 == 0):
                        continue
                    if cg % 2 == 0:
                        S.op("act", lambda e, pr=pr, cl=cl, c=c, j=j, Nt=Nt: e.activation(
                            hT[:, c, :Nt], pT[pr][:, cl, :Nt], AF.Identity,
                            bias=k.mod[:, c, j:j + 1], scale=k.A1[:, c, j:j + 1]),
                            reads=[B_pT[pr], k.B_mod, k.B_A], writes=[B_hT[c]])
                    else:
                        S.op("dve", lambda e, pr=pr, cl=cl, c=c, j=j, Nt=Nt: e.tensor_scalar(
                            hT[:, c, :Nt], pT[pr][:, cl, :Nt], k.A1[:, c, j:j + 1], k.mod[:, c, j:j + 1],
                            op0=ALU.mult, op1=ALU.add),
                            reads=[B_pT[pr], k.B_mod, k.B_A], writes=[B_hT[c]])
            if k.stage < 3:
                continue
            S.dma("sp", cs[:, 0, :Nt], d["cosT"][:, tokcol:tokcol + Nt], writes=[B_cs])
            S.dma("sp", cs[:, 1, :Nt], d["sinT"][:, tokcol:tokcol + Nt], writes=[B_sn])
            outs = [("k", 1024 + 128 * i, i) for i in range(2)] + [("xr", 1536 + 128 * i, i) for i in range(8)]
            if own:
                outs += [("q", 128 * i, i) for i in range(8)] + [("yr", 2560 + 128 * i, i) for i in range(8)]
            for (kind, col, idx) in outs:
                if (k.stage < 4 or k.sub == 4) and kind != "xr":
                    continue
                r = cnt["po"] % NPO
                cnt["po"] += 1
                for kc in range(16):
                    S.op("pe", lambda e, r=r, kc=kc, col=col, Nt=Nt: e.matmul(
                        po[r][:, :Nt], lhsT=win[:, kc, col:col + 128], rhs=hT[:, kc, :Nt],
                        start=(kc == 0), stop=(kc == 15)), reads=[B_win[kc], B_hT[kc]], writes=[B_po[r]])
                if kind == "xr":
                    s = cnt["st"] % NST
                    cnt["st"] += 1
                    S.op("act", lambda e, s=s, r=r, Nt=Nt: e.activation(stg[s][:, :Nt], po[r][:, :Nt], AF.Identity),
                         reads=[B_po[r]], writes=[B_stg[s]])
                    if j == 1:
                        dst = d["XRc"][idx, :, 2:2 + Nt]
                    else:
                        dst = d["XRl"][idx, :, 2 + tokcol - CTX:2 + tokcol - CTX + Nt]
                    S.dma("sp", dst, stg[s][:, :Nt], reads=[B_stg[s]], writes=[sink[s]])
                elif kind == "yr":
                    s = cnt["st"] % NST
                    cnt["st"] += 1
                    S.op("act", lambda e, s=s, r=r, Nt=Nt: e.activation(stg[s][:, :Nt], po[r][:, :Nt],
                                                                        AF.Gelu_apprx_tanh),
                         reads=[B_po[r]], writes=[B_stg[s]])
                    S.dma("sp", d["YGd"][idx, :, tokcol - CTX:tokcol - CTX + Nt], stg[s][:, :Nt],
                          reads=[B_stg[s]], writes=[sink[s]])
                else:
                    gv = k.gq if kind == "q" else k.gk
                    a = cnt["pa"] % 2
                    a2 = (cnt["pa"] + 1) % 2
                    cnt["pa"] += 2
                    S.op("act", lambda e, r=r, Nt=Nt: e.activation(sq[:, :Nt], po[r][:, :Nt], AF.Square),
                         reads=[B_po[r]], writes=[B_sq])
                    S.op("pe", lambda e, a=a, Nt=Nt: e.matmul(pa[a][:, :Nt], lhsT=k.ones_b[:], rhs=sq[:, :Nt],
                                                              start=True, stop=True),
                         reads=[B_sq, k.B_const], writes=[B_pa[a]])
                    S.op("act", lambda e, a=a, Nt=Nt: e.activation(sd[:, :Nt], pa[a][:, :Nt], AF.Sqrt,
                                                                   bias=k.eps_t[:], scale=1.0 / 128),
                         reads=[B_pa[a], k.B_const], writes=[B_sd2])
                    S.op("dve", lambda e, Nt=Nt: e.reciprocal(rinv[:, :Nt], sd[:, :Nt]),
                         reads=[B_sd2], writes=[B_rinv])
                    S.op("dve", lambda e, r=r, Nt=Nt, gv=gv: e.scalar_tensor_tensor(
                        qn[:, :Nt], in0=po[r][:, :Nt], scalar=gv[:, 0:1], in1=rinv[:, :Nt],
                        op0=ALU.mult, op1=ALU.mult), reads=[B_po[r], B_rinv, k.B_g], writes=[B_qn])
                    S.op("pe", lambda e, a2=a2, Nt=Nt: e.matmul(pa[a2][:, :Nt], lhsT=k.perm_b[:], rhs=qn[:, :Nt],
                                                               start=True, stop=True),
                         reads=[B_qn, k.B_const], writes=[B_pa[a2]])
                    S.op("pool", lambda e, Nt=Nt: e.tensor_tensor(t1[:, :Nt], qn[:, :Nt], cs[:, 0, :Nt], ALU.mult),
                         reads=[B_qn, B_cs], writes=[B_t1])
                    S.op("dve", lambda e, a2=a2, Nt=Nt: e.tensor_tensor(t2[:, :Nt], pa[a2][:, :Nt], cs[:, 1, :Nt],
                                                                       ALU.mult),
                         reads=[B_pa[a2], B_sn], writes=[B_t2])
                    s = cnt["sb"] % NST
                    cnt["sb"] += 1
                    S.op("dve", lambda e, s=s, Nt=Nt: e.tensor_tensor(stb[s][:, :Nt], t1[:, :Nt], t2[:, :Nt], ALU.add),
                         reads=[B_t1, B_t2], writes=[B_stb[s]])
                    if kind == "q":
                        dst = d["Qd"][idx, :, tokcol - CTX:tokcol - CTX + Nt]
                    else:
                        dst = d["Kd"][idx, :, tokcol:tokcol + Nt]
                    S.dma("sp", dst, stb[s][:, :Nt], reads=[B_stb[s]], writes=[sinkb[s]])
            for ti in range(nt if k.stage >= 5 else 0):
                r = cnt["po"] % NPO
                cnt["po"] += 1
                for kc in range(16):
                    S.op("pe", lambda e, r=r, kc=kc, ti=ti: e.matmul(
                        po[r][:, 0:256], lhsT=hT[:, kc, ti * 128:(ti + 1) * 128], rhs=win[:, kc, 1280:1536],
                        start=(kc == 0), stop=(kc == 15)), reads=[B_win[kc], B_hT[kc]], writes=[B_po[r]])
                s = cnt["sb"] % NST
                cnt["sb"] += 1
                S.op("act", lambda e, s=s, r=r: e.activation(stb[s][:, 0:256], po[r][:, 0:256], AF.Identity),
                     reads=[B_po[r]], writes=[B_stb[s]])
                S.dma("sp", d["Vd"][tokcol + ti * 128:tokcol + (ti + 1) * 128, :], stb[s][:, 0:256],
                      reads=[B_stb[s]], writes=[sinkb[s]])
            tokcol += Nt
    S.barrier()


def phase_lru(k):
    nc, S, sb, ps = k.nc, k.S, k.sb, k.ps
    d = k.d
    NF = CTX + TOWN
    with ExitStack() as st:
        xr = sb(st, "xr", [128, NTOK + 8], F32)
        B_xr = Buf("xr")
        B_xr2 = Buf("xr2")
        B_mixs = Buf("mixsink")
        u = sb(st, "u", [128, NTOK], F32)
        B_u = Buf("u")
        ub = sb(st, "ub", [128, NTOK], BF16)
        B_ub = Buf("ub")
        rr = sb(st, "rr", [128, NTOK], F32)
        B_rr = Buf("rr")
        ii = sb(st, "ii", [128, NTOK], F32)
        B_ii = Buf("ii")
        aa = sb(st, "aa", [128, NTOK], F32)
        B_aa = Buf("aa")
        mm = sb(st, "mm", [128, NTOK], F32)
        B_mm = Buf("mm")
        hf = sb(st, "hf", [128, NF], F32)
        B_hf = Buf("hf")
        hb = sb(st, "hb", [128, NTOK], F32)
        B_hb = Buf("hb")
        yg = sb(st, "yg", [128, TOWN], F32)
        B_yg = Buf("yg")
        wg = sb(st, "wg", [128, 2, 2, 128], BF16)
        B_wg = Buf("wg")
        pg = [ps(st, f"pg{i}", [128, 512], F32) for i in range(4)]
        B_pg = [Buf(f"pg{i}") for i in range(4)]
        lv = k.lruv
        sg = sb(st, "sg", [128, 2, 8], F32)
        B_sg = Buf("sg")
        S.op("act", lambda e: e.activation(sg[:], lv[:, :, :, 2], AF.Sigmoid), reads=[k.B_g], writes=[B_sg])
        S.op("act", lambda e: e.activation(sg[:], sg[:], AF.Ln), reads=[B_sg], writes=[B_sg])
        S.op("dve", lambda e: e.tensor_scalar(lv[:, :, :, 3], sg[:], 8.0, None, op0=ALU.mult),
             reads=[B_sg], writes=[k.B_lv])
        S.op("dve", lambda e: e.tensor_scalar(lv[:, :, :, 4], sg[:], 16.0, None, op0=ALU.mult),
             reads=[B_sg], writes=[k.B_lv])
        pcnt = 0
        for n in range(8):
            S.dma("sp", xr[:, 0:CTX + 4], d["XRc"][n], writes=[B_xr])
            S.dma("sp", xr[:, CTX + 4:], d["XRl"][n], writes=[B_xr2])
            S.dma("sp", yg[:], d["YGd"][n], writes=[B_yg])
            S.dma("pool", wg[:], d["wgate"][n], writes=[B_wg])
            for (o0, u0, L) in ((0, 0, CTX), (CTX + 4, CTX, SEQ)):
                S.op("dve", lambda e, o0=o0, u0=u0, L=L, n=n: e.tensor_scalar(
                    u[:, u0:u0 + L], xr[:, o0:o0 + L], k.convw[:, n, 0:1], k.convw[:, n, 5:6],
                    op0=ALU.mult, op1=ALU.add), reads=[B_xr, B_xr2, k.B_g], writes=[B_u])
                for tap in range(1, 5):
                    S.op("dve", lambda e, o0=o0, u0=u0, L=L, n=n, tap=tap: e.scalar_tensor_tensor(
                        u[:, u0:u0 + L], in0=xr[:, o0 + tap:o0 + tap + L], scalar=k.convw[:, n, tap:tap + 1],
                        in1=u[:, u0:u0 + L], op0=ALU.mult, op1=ALU.add), reads=[B_xr, B_xr2, B_u, k.B_g], writes=[B_u])
            S.op("act", lambda e: e.activation(ub[:], u[:], AF.Identity), reads=[B_u], writes=[B_ub])
            for dr in range(2):
                N = NF if dr == 0 else NTOK
                blks = [(c0, min(512, N - c0)) for c0 in range(0, N, 512)]
                for gi, dst, B_dst in ((0, rr, B_rr), (1, ii, B_ii)):
                    for (c0, w) in blks:
                        p = pcnt % 4
                        pcnt += 1
                        S.op("pe", lambda e, p=p, c0=c0, w=w, dr=dr, gi=gi: e.matmul(
                            pg[p][:, :w], lhsT=wg[:, dr, gi, :], rhs=ub[:, c0:c0 + w], start=True, stop=True),
                            reads=[B_wg, B_ub], writes=[B_pg[p]])
                        S.op("act", lambda e, p=p, c0=c0, w=w, dr=dr, gi=gi, dst=dst, n=n: e.activation(
                            dst[:, c0:c0 + w], pg[p][:, :w], AF.Sigmoid, bias=lv[:, dr, n, gi:gi + 1]),
                            reads=[B_pg[p], k.B_g], writes=[B_dst])
                S.op("act", lambda e, N=N, dr=dr, n=n: e.activation(aa[:, :N], rr[:, :N], AF.Exp,
                                                                    scale=lv[:, dr, n, 3:4]),
                     reads=[B_rr, k.B_lv], writes=[B_aa])
                S.op("act", lambda e, N=N, dr=dr, n=n: e.activation(mm[:, :N], rr[:, :N], AF.Exp,
                                                                    scale=lv[:, dr, n, 4:5]),
                     reads=[B_rr, k.B_lv], writes=[B_mm])
                S.op("dve", lambda e, N=N: e.tensor_scalar(mm[:, :N], mm[:, :N], -1.0, 1.0,
                                                           op0=ALU.mult, op1=ALU.add),
                     reads=[B_mm], writes=[B_mm])
                S.op("act", lambda e, N=N: e.activation(mm[:, :N], mm[:, :N], AF.Sqrt), reads=[B_mm], writes=[B_mm])
                S.op("dve", lambda e, N=N: e.tensor_tensor(ii[:, :N], ii[:, :N], mm[:, :N], ALU.mult),
                     reads=[B_ii, B_mm], writes=[B_ii])
                S.op("dve", lambda e, N=N: e.tensor_tensor(ii[:, :N], ii[:, :N], u[:, :N], ALU.mult),
                     reads=[B_ii, B_u], writes=[B_ii])
                if dr == 0:
                    S.op("dve", lambda e: e.tensor_tensor_scan(hf[:, :], data0=aa[:, :NF], data1=ii[:, :NF],
                                                               initial=0.0, op0=ALU.mult, op1=ALU.add),
                         reads=[B_aa, B_ii], writes=[B_hf])
                else:
                    S.op("dve", lambda e: e.tensor_tensor_scan(
                        hb[:, 0:CTX][:, ::-1], data0=aa[:, 0:CTX][:, ::-1], data1=ii[:, 0:CTX][:, ::-1],
                        initial=0.0, op0=ALU.mult, op1=ALU.add), reads=[B_aa, B_ii], writes=[B_hb])
                    S.op("dve", lambda e: e.tensor_tensor_scan(
                        hb[:, CTX:NTOK][:, ::-1], data0=aa[:, CTX:NTOK][:, ::-1], data1=ii[:, CTX:NTOK][:, ::-1],
                        initial=hb[:, 0:1], op0=ALU.mult, op1=ALU.add), reads=[B_aa, B_ii, B_hb], writes=[B_hb])
            S.op("dve", lambda e: e.tensor_tensor(hf[:, CTX:NF], hf[:, CTX:NF], hb[:, CTX:NF], ALU.add),
                 reads=[B_hf, B_hb], writes=[B_hf])
            S.op("dve", lambda e: e.tensor_tensor(yg[:], yg[:], hf[:, CTX:NF], ALU.mult),
                 reads=[B_hf, B_yg], writes=[B_yg])
            S.dma("sp", d["MIXd"][8 + n], yg[:], reads=[B_yg], writes=[B_mixs])
    S.barrier()


def phase_attn(k):
    nc, S, sb, ps = k.nc, k.S, k.sb, k.ps
    d = k.d
    NCH = NTOK // 128
    with ExitStack() as st:
        kT = sb(st, "kT", [128, 2, NTOK], BF16)
        B_kT = Buf("kT")
        vv = sb(st, "vv", [128, NCH, 256], BF16)
        B_vv = Buf("vv")
        qT = [sb(st, f"qT{i}", [128, 8, 512], BF16) for i in range(2)]
        B_qT = [Buf(f"qT{i}") for i in range(2)]
        NP = 4
        pt = [sb(st, f"pt{i}", [128, 512], BF16) for i in range(NP)]
        B_pt = [Buf(f"pt{i}") for i in range(NP)]
        NS = 3
        pss = [ps(st, f"pss{i}", [128, 512], F32) for i in range(NS)]
        B_pss = [Buf(f"pss{i}") for i in range(NS)]
        pso = [ps(st, f"pso{i}", [128, 512], F32) for i in range(2)]
        B_pso = [Buf(f"pso{i}") for i in range(2)]
        psl = [ps(st, f"psl{i}", [128, 512], F32) for i in range(2)]
        B_psl = [Buf(f"psl{i}") for i in range(2)]
        rl = sb(st, "rl", [128, 512], F32)
        B_rl = Buf("rl")
        ao = [sb(st, f"ao{i}", [128, 512], F32) for i in range(2)]
        B_ao = [Buf(f"ao{i}") for i in range(2)]
        sink = [Buf(f"asink{i}") for i in range(2)]
        S.dma("sp", kT[:], d["Kd"].rearrange("h p t -> p h t"), writes=[B_kT])
        S.dma("sp", vv[:], d["Vd"].rearrange("(c p) f -> p c f", p=128), writes=[B_vv])
        sc = 128.0 ** -0.5
        it = 0
        scnt = 0
        for qb in range(4):
            S.dma("sp", qT[qb % 2][:], d["Qd"].rearrange("h p t -> p h t")[:, :, qb * 512:(qb + 1) * 512],
                  writes=[B_qT[qb % 2]])
            for h in range(8):
                kvh = h // 4
                o = it % 2
                it += 1

                def smm(c, h=h, kvh=kvh, qb=qb):
                    nonlocal scnt
                    s = scnt % NS
                    scnt += 1
                    S.op("pe", lambda e, s=s, c=c: e.matmul(
                        pss[s][:], lhsT=kT[:, kvh, c * 128:(c + 1) * 128], rhs=qT[qb % 2][:, h, :],
                        start=True, stop=True), reads=[B_kT, B_qT[qb % 2]], writes=[B_pss[s]])
                    return s
                s_next = smm(0)
                for c in range(NCH):
                    s_cur = s_next
                    if c + 1 < NCH:
                        s_next = smm(c + 1)
                    p = (it * NCH + c) % NP
                    S.op("act", lambda e, p=p, s=s_cur: e.activation(pt[p][:], pss[s][:], AF.Exp, scale=sc),
                         reads=[B_pss[s_cur]], writes=[B_pt[p]])
                    S.op("pe", lambda e, p=p, c=c, o=o, kvh=kvh: e.matmul(
                        pso[o][:], lhsT=vv[:, c, kvh * 128:(kvh + 1) * 128], rhs=pt[p][:],
                        start=(c == 0), stop=(c == NCH - 1)), reads=[B_vv, B_pt[p]], writes=[B_pso[o]])
                    S.op("pe", lambda e, p=p, c=c, o=o: e.matmul(
                        psl[o][:], lhsT=k.ones_b[:], rhs=pt[p][:], start=(c == 0), stop=(c == NCH - 1)),
                        reads=[k.B_const, B_pt[p]], writes=[B_psl[o]])
                S.op("dve", lambda e, o=o: e.reciprocal(rl[:], psl[o][:]), reads=[B_psl[o]], writes=[B_rl])
                S.op("dve", lambda e, o=o: e.tensor_tensor(ao[o][:], pso[o][:], rl[:], ALU.mult),
                     reads=[B_pso[o], B_rl], writes=[B_ao[o]])
                S.dma("sp", d["MIXd"][h, :, qb * 512:(qb + 1) * 512], ao[o][:], reads=[B_ao[o]], writes=[sink[o]])
    S.barrier()


def phase_out(k):
    nc, S, sb, ps = k.nc, k.S, k.sb, k.ps
    d = k.d
    with ExitStack() as st:
        wo = sb(st, "wo", [128, 16, D], BF16)
        B_wo = [Buf(f"wo{i}") for i in range(16)]
        for c in range(16):
            S.dma("pool", wo[:, c, :], d["w_out"][c * 128:(c + 1) * 128, :], writes=[B_wo[c]])
        wr = sb(st, "wr", [128, 16, NE], F32)
        B_wr = Buf("wr")
        S.dma("sp", wr[:], d["w_router"].rearrange("(c p) e -> p c e", p=128), writes=[B_wr])
        br = sb(st, "br", [128, NE], F32)
        B_br = Buf("br")
        S.dma("sp", br[:], d["b_router_b"], writes=[B_br])
        gt1 = sb(st, "gt1", [128, D], F32)
        B_gt1 = Buf("gt1")
        S.dma("sp", gt1[:], d["MODd_b"][2], writes=[B_gt1])
        mx = sb(st, "mx", [128, 16, 512], F32)
        B_mx = [Buf(f"mx{i}") for i in range(16)]
        sq = sb(st, "sq2", [128, 512], BF16)
        B_sq = Buf("sq2")
        sd = sb(st, "sdo", [128, 512], F32)
        B_sd = Buf("sdo")
        rinv = sb(st, "rinvo", [128, 2, 512], F32)
        B_rinv = [Buf("rinv0"), Buf("rinv1")]
        mn = sb(st, "mn", [128, 16, 512], BF16)
        B_mn = [Buf(f"mn{i}") for i in range(16)]
        pss = ps(st, "pss", [128, 512], F32)
        B_pss = Buf("pss")
        pw = [ps(st, f"pw{i}", [128, 512], F32) for i in range(3)]
        B_pw = [Buf(f"pw{i}") for i in range(3)]
        ptr = [ps(st, f"ptr{i}", [128, 4, 128], F32) for i in range(2)]
        B_ptr = [Buf(f"ptr{i}") for i in range(2)]
        prt = ps(st, "prt", [128, NE], F32)
        B_prt = Buf("prt")
        pgt = ps(st, "pgt", [NE, 128], F32)
        B_pgt = Buf("pgt")
        xo = sb(st, "xo", [128, D], F32)
        B_xo = Buf("xo")
        x1 = sb(st, "x1", [128, D], F32)
        B_x1 = Buf("x1")
        junk = sb(st, "junk2", [128, D], BF16)
        B_junk = Buf("junk2")
        stat = sb(st, "stat2", [128, 8], F32)
        B_st = [Buf(f"st2{i}") for i in range(8)]
        xn2 = sb(st, "xn2", [128, D], F32)
        B_xn2 = Buf("xn2")
        h2f = sb(st, "h2f", [128, 16, 128], F32)
        B_h2f = Buf("h2f")
        h2b = sb(st, "h2b", [128, 16, 128], BF16)
        B_h2b = Buf("h2b")
        lg = sb(st, "lg", [128, NE], F32)
        B_lg = Buf("lg")
        t8 = sb(st, "t8", [128, 8], F32)
        B_t8 = Buf("t8")
        msk = sb(st, "msk", [128, NE], F32)
        B_msk = Buf("msk")
        ex = sb(st, "ex", [128, NE], F32)
        B_ex = Buf("ex")
        gg = sb(st, "gg", [128, NE], F32)
        B_gg = Buf("gg")
        gts = sb(st, "gts", [NE, 128], F32)
        B_gts = Buf("gts")
        B_x1s, B_h2s, B_gs, B_gtsk = Buf("x1sink"), Buf("h2sink"), Buf("gsink"), Buf("gtsink")
        pwc = 0
        for tb in range(4):
            for c in range(16):
                S.dma("sp", mx[:, c, :], d["MIXd"][c, :, tb * 512:(tb + 1) * 512], writes=[B_mx[c]])
            for g in range(2):
                for cl in range(8):
                    c = g * 8 + cl
                    S.op("act", lambda e, c=c: e.activation(sq[:], mx[:, c, :], AF.Square),
                         reads=[B_mx[c]], writes=[B_sq])
                    S.op("pe", lambda e, cl=cl: e.matmul(pss[:], lhsT=k.ones_b[:], rhs=sq[:],
                                                         start=(cl == 0), stop=(cl == 7)),
                         reads=[B_sq, k.B_const], writes=[B_pss])
                S.op("act", lambda e: e.activation(sd[:], pss[:], AF.Sqrt, bias=k.eps_t[:], scale=1.0 / 1024),
                     reads=[B_pss, k.B_const], writes=[B_sd])
                S.op("dve", lambda e, g=g: e.reciprocal(rinv[:, g, :], sd[:]), reads=[B_sd], writes=[B_rinv[g]])
                for cl in range(8):
                    c = g * 8 + cl
                    S.op("dve", lambda e, c=c, g=g: e.scalar_tensor_tensor(
                        mn[:, c, :], in0=mx[:, c, :], scalar=k.gmix[:, c:c + 1], in1=rinv[:, g, :],
                        op0=ALU.mult, op1=ALU.mult), reads=[B_mx[c], B_rinv[g], k.B_g], writes=[B_mn[c]])
            for tt in range(4):
                tg = tb * 4 + tt
                S.dma("sp", xo[:], d["x_all"][tg * 128:(tg + 1) * 128, :], writes=[B_xo])
                for dg in range(4):
                    p = pwc % 3
                    pwc += 1
                    for c in range(16):
                        S.op("pe", lambda e, p=p, c=c, tt=tt, dg=dg: e.matmul(
                            pw[p][:], lhsT=mn[:, c, tt * 128:(tt + 1) * 128], rhs=wo[:, c, dg * 512:(dg + 1) * 512],
                            start=(c == 0), stop=(c == 15)), reads=[B_mn[c], B_wo[c]], writes=[B_pw[p]])
                    S.op("dve", lambda e, p=p, dg=dg: e.tensor_tensor(
                        x1[:, dg * 512:(dg + 1) * 512], pw[p][:], gt1[:, dg * 512:(dg + 1) * 512], ALU.mult),
                        reads=[B_pw[p], B_gt1], writes=[B_x1])
                S.op("pool", lambda e: e.tensor_tensor(x1[:], x1[:], xo[:], ALU.add), reads=[B_x1, B_xo], writes=[B_x1])
                S.dma("sp", d["X1d"][tg * 128:(tg + 1) * 128, :], x1[:], reads=[B_x1], writes=[B_x1s])
                q = tg % 2
                S.op("act", lambda e, q=q: e.activation(junk[:], x1[:], AF.Square, accum_out=stat[:, q:q + 1]),
                     reads=[B_x1], writes=[B_junk, B_st[q]])
                S.op("act", lambda e, q=q: e.activation(stat[:, 2 + q:3 + q], stat[:, q:q + 1], AF.Sqrt,
                                                        bias=k.eps_t[:], scale=1.0 / D),
                     reads=[B_st[q], k.B_const], writes=[B_st[2 + q]])
                S.op("dve", lambda e, q=q: e.reciprocal(stat[:, 4 + q:5 + q], stat[:, 2 + q:3 + q]),
                     reads=[B_st[2 + q]], writes=[B_st[4 + q]])
                S.op("dve", lambda e, q=q: e.tensor_scalar(xn2[:], x1[:], stat[:, 4 + q:5 + q], None, op0=ALU.mult),
                     reads=[B_x1, B_st[4 + q]], writes=[B_xn2])
                for cg in range(4):
                    pr = cg % 2
                    for cl in range(4):
                        c = cg * 4 + cl
                        S.op("pe", lambda e, pr=pr, cl=cl, c=c: e.transpose(
                            ptr[pr][:, cl, :], xn2[:, c * 128:(c + 1) * 128], k.ident_f[:]),
                            reads=[B_xn2, k.B_const], writes=[B_ptr[pr]])
                    for cl in range(4):
                        c = cg * 4 + cl
                        S.op("act", lambda e, pr=pr, cl=cl, c=c: e.activation(
                            h2f[:, c, :], ptr[pr][:, cl, :], AF.Identity, bias=k.mod[:, 48 + c, 0:1],
                            scale=k.A2[:, c:c + 1]), reads=[B_ptr[pr], k.B_mod, k.B_A], writes=[B_h2f])
                S.op("dve", lambda e: e.tensor_copy(h2b[:], h2f[:]), reads=[B_h2f], writes=[B_h2b])
                S.dma("sp", d["H2Td"].rearrange("c p t -> p c t")[:, :, tg * 128:(tg + 1) * 128], h2b[:],
                      reads=[B_h2b], writes=[B_h2s])
                for c in range(16):
                    S.op("pe", lambda e, c=c: e.matmul(prt[:], lhsT=h2f[:, c, :], rhs=wr[:, c, :],
                                                       start=(c == 0), stop=(c == 15)),
                         reads=[B_h2f, B_wr], writes=[B_prt])
                S.op("dve", lambda e: e.tensor_tensor(lg[:], prt[:], br[:], ALU.add), reads=[B_prt, B_br], writes=[B_lg])
                S.op("dve", lambda e: e.max(out=t8[:], in_=lg[:]), reads=[B_lg], writes=[B_t8])
                S.op("dve", lambda e: e.tensor_scalar(msk[:], lg[:], t8[:, 3:4], None, op0=ALU.is_ge),
                     reads=[B_lg, B_t8], writes=[B_msk])
                S.op("dve", lambda e: e.tensor_scalar(t8[:, 7:8], t8[:, 0:1], -1.0, None, op0=ALU.mult),
                     reads=[B_t8], writes=[B_t8])
                S.op("act", lambda e: e.activation(ex[:], lg[:], AF.Exp, bias=t8[:, 7:8]),
                     reads=[B_lg, B_t8], writes=[B_ex])
                S.op("dve", lambda e: e.tensor_tensor(ex[:], ex[:], msk[:], ALU.mult), reads=[B_ex, B_msk], writes=[B_ex])
                S.op("dve", lambda e: e.reduce_sum(t8[:, 6:7], ex[:], axis=mybir.AxisListType.X),
                     reads=[B_ex], writes=[B_t8])
                S.op("dve", lambda e: e.reciprocal(t8[:, 5:6], t8[:, 6:7]), reads=[B_t8], writes=[B_t8])
                S.op("dve", lambda e: e.tensor_scalar(gg[:], ex[:], t8[:, 5:6], None, op0=ALU.mult),
                     reads=[B_ex, B_t8], writes=[B_gg])
                S.dma("sp", d["Gd"][tg * 128:(tg + 1) * 128, :], gg[:], reads=[B_gg], writes=[B_gs])
                S.op("pe", lambda e: e.transpose(pgt[:], gg[:], k.ident_f[:]), reads=[B_gg, k.B_const], writes=[B_pgt])
                S.op("act", lambda e: e.activation(gts[:], pgt[:], AF.Identity), reads=[B_pgt], writes=[B_gts])
                S.dma("sp", d["GTd"][:, tg * 128:(tg + 1) * 128], gts[:], reads=[B_gts], writes=[B_gtsk])
    S.barrier()


def phase_moe(k, n_exp=NE):
    nc, S, sb, ps = k.nc, k.S, k.sb, k.ps
    d = k.d
    TH = 1024
    with ExitStack() as st:
        acc = sb(st, "acc", [128, 8, D], F32)
        B_acc = [Buf(f"acc{i}") for i in range(8)]
        h2 = sb(st, "h2", [128, 16, TH], BF16)
        B_h2 = Buf("h2")
        aT = sb(st, "aT", [128, 16, TH], BF16)
        B_aT = [Buf(f"aT{i}") for i in range(16)]
        NG = 2
        wgu = [sb(st, f"wgu{i}", [128, 16, 256], BF16) for i in range(NG)]
        B_wgu = [Buf(f"wgu{i}") for i in range(NG)]
        wdn = [sb(st, f"wdn{i}", [128, 16, 256], BF16) for i in range(NG)]
        B_wdn = [Buf(f"wdn{i}") for i in range(NG)]
        G = sb(st, "G", [128, 16, NE], F32)
        B_G = Buf("G")
        GT = sb(st, "GT", [NE, TOWN], F32)
        B_GT = Buf("GT")
        bdn = sb(st, "bdn", [NE, D], F32)
        B_bdn = Buf("bdn")
        bgu = sb(st, "bgu", [128, NE, 16, 2], F32)
        B_bgu = Buf("bgu")
        NT = 2
        tg_ = [sb(st, f"tg{i}", [128, 512], F32) for i in range(NT)]
        ts_ = [sb(st, f"ts{i}", [128, 512], F32) for i in range(NT)]
        tu_ = [sb(st, f"tu{i}", [128, 512], F32) for i in range(NT)]
        B_tg = [Buf(f"tg{i}") for i in range(NT)]
        B_ts = [Buf(f"ts{i}") for i in range(NT)]
        B_tu = [Buf(f"tu{i}") for i in range(NT)]
        pgu = [ps(st, f"pgu{i}", [128, 512], F32) for i in range(4)]
        B_pgu = [Buf(f"pgu{i}") for i in range(4)]
        py = [ps(st, f"py{i}", [128, 512], F32) for i in range(4)]
        B_py = [Buf(f"py{i}") for i in range(4)]
        S.dma("sp", G[:], d["Gd"].rearrange("(t p) e -> p t e", p=128), writes=[B_G])
        S.dma("sp", GT[:], d["GTd"], writes=[B_GT])
        S.dma("sp", bdn[:], d["b_down"], writes=[B_bdn])
        S.dma("sp", bgu[:], d["bgu_l"], writes=[B_bgu])
        cp = 0
        cy = 0
        ct = 0
        jobs = []
        for hf in range(2):
            for ex in range(n_exp):
                jobs += [("gu", hf, ex, jc) for jc in range(16)]
                jobs += [("dn", hf, ex, dg) for dg in range(8)]
        ring = {"gu": 0, "dn": 0}
        slot_of = {}

        def issue(i):
            kind, hf_, ex_, idx = jobs[i]
            w = ring[kind] % NG
            ring[kind] += 1
            slot_of[i] = w
            if kind == "gu":
                src = d["w_gate_up"][ex_].rearrange("(kc p) f -> p kc f", p=128)[:, :, idx * 256:(idx + 1) * 256]
                S.dma("pool", wgu[w][:], src, writes=[B_wgu[w]])
            else:
                src = d["w_down"][ex_].rearrange("(kc p) f -> p kc f", p=128)[:, :, idx * 256:(idx + 1) * 256]
                S.dma("pool", wdn[w][:], src, writes=[B_wdn[w]])
        issue(0)
        ji = 0
        for hf in range(2):
            S.dma("sp", h2[:], d["H2Td"].rearrange("c p t -> p c t")[:, :, hf * TH:(hf + 1) * TH], writes=[B_h2])
            for tt in range(8):
                tgl = hf * 8 + tt
                for dg in range(4):
                    p = cy % 4
                    cy += 1
                    S.op("pe", lambda e, p=p, tgl=tgl, dg=dg: e.matmul(
                        py[p][:], lhsT=GT[:, tgl * 128:(tgl + 1) * 128], rhs=bdn[:, dg * 512:(dg + 1) * 512],
                        start=True, stop=True), reads=[B_GT, B_bdn], writes=[B_py[p]])
                    S.op("act", lambda e, p=p, tt=tt, dg=dg: e.activation(acc[:, tt, dg * 512:(dg + 1) * 512], py[p][:], AF.Identity),
                         reads=[B_py[p]], writes=[B_acc[tt]])
            for ex in range(n_exp):
                for jc in range(16):
                    w = slot_of[ji]
                    if ji + 1 < len(jobs):
                        issue(ji + 1)
                    ji += 1
                    for tb in range(2):
                        pg_, pu_ = cp % 4, (cp + 1) % 4
                        cp += 2
                        for (pp, off) in ((pg_, 0), (pu_, 1)):
                            for kc in range(16):
                                S.op("pe", lambda e, pp=pp, off=off, kc=kc, w=w, tb=tb: e.matmul(
                                    pgu[pp][:], lhsT=wgu[w][:, kc, off::2], rhs=h2[:, kc, tb * 512:(tb + 1) * 512],
                                    start=(kc == 0), stop=(kc == 15)), reads=[B_wgu[w], B_h2], writes=[B_pgu[pp]])
                        t = ct % NT
                        ct += 1
                        S.op("dve", lambda e, t=t, pg_=pg_, ex=ex, jc=jc: e.tensor_scalar(
                            tg_[t][:], pgu[pg_][:], bgu[:, ex, jc, 0:1], 7.0, op0=ALU.add, op1=ALU.min),
                            reads=[B_pgu[pg_], B_bgu], writes=[B_tg[t]])
                        S.op("act", lambda e, t=t: e.activation(ts_[t][:], tg_[t][:], AF.Sigmoid, scale=1.702),
                             reads=[B_tg[t]], writes=[B_ts[t]])
                        S.op("dve", lambda e, t=t, pu_=pu_, ex=ex, jc=jc: e.tensor_scalar(
                            tu_[t][:], pgu[pu_][:], bgu[:, ex, jc, 1:2], -7.0, op0=ALU.add, op1=ALU.max),
                            reads=[B_pgu[pu_], B_bgu], writes=[B_tu[t]])
                        S.op("dve", lambda e, t=t: e.tensor_scalar(tu_[t][:], tu_[t][:], 7.0, 1.0,
                                                                   op0=ALU.min, op1=ALU.add),
                             reads=[B_tu[t]], writes=[B_tu[t]])
                        S.op("dve", lambda e, t=t: e.tensor_tensor(tg_[t][:], tg_[t][:], ts_[t][:], ALU.mult),
                             reads=[B_tg[t], B_ts[t]], writes=[B_tg[t]])
                        S.op("dve", lambda e, t=t, jc=jc, tb=tb: e.tensor_tensor(
                            aT[:, jc, tb * 512:(tb + 1) * 512], tg_[t][:], tu_[t][:], ALU.mult),
                            reads=[B_tg[t], B_tu[t]], writes=[B_aT[jc]])
                for dg in range(8):
                    w = slot_of[ji]
                    if ji + 1 < len(jobs):
                        issue(ji + 1)
                    ji += 1
                    for tt in range(8):
                        p = cy % 4
                        cy += 1
                        tgl = hf * 8 + tt
                        for jc in range(16):
                            S.op("pe", lambda e, p=p, jc=jc, tt=tt, w=w: e.matmul(
                                py[p][:, 0:256], lhsT=aT[:, jc, tt * 128:(tt + 1) * 128], rhs=wdn[w][:, jc, :],
                                start=(jc == 0), stop=(jc == 15)), reads=[B_aT[jc], B_wdn[w]], writes=[B_py[p]])
                        S.op("dve", lambda e, p=p, tt=tt, dg=dg, tgl=tgl, ex=ex: e.scalar_tensor_tensor(
                            acc[:, tt, dg * 256:(dg + 1) * 256], in0=py[p][:, 0:256], scalar=G[:, tgl, ex:ex + 1],
                            in1=acc[:, tt, dg * 256:(dg + 1) * 256], op0=ALU.mult, op1=ALU.add),
                            reads=[B_py[p], B_G, B_acc[tt]], writes=[B_acc[tt]])
            chunks = [(tt, dg) for tt in range(8) for dg in range(4)]

            def fin_load(i):
                tt, dg = chunks[i]
                tgl = hf * 8 + tt
                t = (ct + i) % NT
                sl = slice(dg * 512, (dg + 1) * 512)
                S.dma("sp", tg_[t][:], d["X1d"][tgl * 128:(tgl + 1) * 128, sl], writes=[B_tg[t]])
                S.dma("sp", ts_[t][:], d["MODd_b"][5][:, sl], writes=[B_ts[t]])
            fin_load(0)
            for i, (tt, dg) in enumerate(chunks):
                tgl = hf * 8 + tt
                t = (ct + i) % NT
                sl = slice(dg * 512, (dg + 1) * 512)
                if i + 1 < len(chunks):
                    fin_load(i + 1)
                S.op("dve", lambda e, t=t, tt=tt, sl=sl: e.tensor_tensor(tu_[t][:], acc[:, tt, sl], ts_[t][:], ALU.mult),
                     reads=[B_acc[tt], B_ts[t]], writes=[B_tu[t]])
                S.op("dve", lambda e, t=t: e.tensor_tensor(tu_[t][:], tu_[t][:], tg_[t][:], ALU.add),
                     reads=[B_tu[t], B_tg[t]], writes=[B_tu[t]])
                if len(k.out_bufs) < 4:
                    k.out_bufs.append(Buf(f"outsink{len(k.out_bufs)}"))
                ob = k.out_bufs[(tgl * 4 + dg) % 4]
                S.dma("sp", d["out"][tgl * 128:(tgl + 1) * 128, sl], tu_[t][:], reads=[B_tu[t]], writes=[ob])
            ct += len(chunks)
    S.op("sp", lambda e: e.nop(), reads=k.out_bufs)


IN_SPECS = {
    "x_all": ([SEQ, D], F32), "ctx_l": ([CTX, D], F32), "cT": ([128, 16, 2], F32),
    "w_mod": ([D, 6 * D], F32), "bm_row": ([2, 6 * D], F32), "gvec": ([128, 16, 4], F32),
    "w_in": ([D, 3584], F32), "gqk": ([128, 2], F32), "convw": ([128, 8, 6], F32),
    "wgate": ([8, 128, 2, 2, 128], F32), "lruv": ([128, 2, 8, 5], F32),
    "w_out": ([D, D], F32), "w_router": ([D, NE], F32), "b_router_b": ([128, NE], F32),
    "w_gate_up": ([NE, D, 2 * D], F32), "bgu_l": ([128, NE, 16, 2], F32),
    "w_down": ([NE, D, D], F32), "b_down": ([NE, D], F32),
    "cosT": ([128, NTOK], F32), "sinT": ([128, NTOK], F32),
    "ident_f": ([128, 128], F32), "cb": ([128, 3, 128], BF16),
}
SCRATCH = {
    "MODd": ([96, 128], F32), "XRc": ([8, 128, CTX + 4], F32), "XRl": ([8, 128, SEQ + 4], F32),
    "YGd": ([8, 128, TOWN], F32), "Qd": ([8, 128, TOWN], BF16), "Kd": ([2, 128, NTOK], BF16),
    "Vd": ([NTOK, 256], BF16), "MIXd": ([16, 128, TOWN], F32), "X1d": ([TOWN, D], F32),
    "H2Td": ([16, 128, TOWN], BF16), "Gd": ([TOWN, NE], F32), "GTd": ([NE, TOWN], F32),
}


def build(phases="0ALBOM", dbg_in=(), dbg_out=(), n_exp=NE, skip=(), nblk=9, stage=9, sub=0):
    nc = bass.Bass("TRN2", target_bir_lowering=False)
    k = K()
    k.nblk = nblk
    k.stage = stage
    k.sub = sub
    k.nc = nc
    k.S = Sched(nc)
    _mk(k)
    d = {}
    for n, (shp, dt) in IN_SPECS.items():
        if n in skip:
            continue
        d[n] = nc.dram_tensor(n, shp, dt, kind="ExternalInput").ap()
    for n, (shp, dt) in SCRATCH.items():
        kind = "ExternalInput" if n in dbg_in else ("ExternalOutput" if n in dbg_out else "Internal")
        d[n] = nc.dram_tensor(n, shp, dt, kind=kind).ap()
    d["out"] = nc.dram_tensor("out", [TOWN, D], F32, kind="ExternalOutput").ap()
    md = d["MODd"]
    d["MODd_b"] = [md[s * 16:(s + 1) * 16, :].rearrange("c f -> (c f)").partition_broadcast(128) for s in range(6)]
    k.d = d
    k.out_bufs = []
    S = k.S
    with ExitStack() as st:
        sb = k.sb
        k.ident_f = sb(st, "ident_f", [128, 128], F32)
        cb = sb(st, "cb", [128, 3, 128], BF16)
        k.ident_b, k.ones_b, k.perm_b = cb[:, 0, :], cb[:, 1, :], cb[:, 2, :]
        k.eps_t = sb(st, "eps_t", [128, 1], F32)
        k.B_const = Buf("const")
        k.mod = sb(st, "mod", [128, 96, 2], F32)
        k.B_mod = Buf("mod")
        k.A1 = sb(st, "A1", [128, 16, 2], F32)
        k.A2 = sb(st, "A2", [128, 16], F32)
        k.B_A = Buf("A")
        gvec = sb(st, "gvec", [128, 16, 4], F32)
        k.g1, k.g2, k.gmix = gvec[:, :, 0], gvec[:, :, 1], gvec[:, :, 2]
        gqk = sb(st, "gqk", [128, 2], F32)
        k.gq, k.gk = gqk[:, 0:1], gqk[:, 1:2]
        k.convw = sb(st, "convw", [128, 8, 6], F32)
        k.lruv = sb(st, "lruv", [128, 2, 8, 5], F32)
        k.B_g = Buf("g")
        k.B_lv = Buf("lv")
        S.dma("sp", k.ident_f[:], d["ident_f"], writes=[k.B_const])
        S.dma("sp", cb[:], d["cb"], writes=[Buf("cb")])
        S.op("dve", lambda e: e.memset(k.eps_t[:], EPS), writes=[Buf("epsb")])
        S.dma("sp", gvec[:], d["gvec"], writes=[k.B_g])
        S.dma("sp", gqk[:], d["gqk"], writes=[Buf("gqk")])
        S.dma("sp", k.convw[:], d["convw"], writes=[Buf("cw")])
        S.dma("sp", k.lruv[:], d["lruv"], writes=[k.B_lv])
        S.barrier()
        if "0" in phases:
            phase_mod(k)
        if "A" in phases:
            phase_proj(k)
        if "L" in phases:
            phase_lru(k)
        if "B" in phases:
            phase_attn(k)
        if "O" in phases:
            phase_out(k)
        if "M" in phases:
            phase_moe(k, n_exp)
        S.emit()
    return nc


def _rope_tables(half):
    GRID_W = 64
    pos = np.arange(SEQ)
    if half == 1:
        pos = pos[::-1]
    row = (pos // GRID_W).astype(np.float32)
    col = (pos % GRID_W).astype(np.float32)
    inv = (10000.0 ** (-np.arange(32, dtype=np.float32) / 32)).astype(np.float32)
    cosT = np.ones((128, NTOK), np.float32)
    sinT = np.zeros((128, NTOK), np.float32)
    for dd in range(128):
        ax = dd // 64
        hh = (dd % 64) // 32
        f = dd % 32
        ang = (row if ax == 0 else col) * inv[f]
        cosT[dd, CTX:] = np.cos(ang)
        sinT[dd, CTX:] = np.sin(ang) * (-1.0 if hh == 0 else 1.0)
    return cosT, sinT


def _consts():
    ident = np.eye(128, dtype=np.float32)
    perm = np.zeros((128, 128), np.float32)
    for dd in range(128):
        p = dd + 32 if (dd % 64) < 32 else dd - 32
        perm[p, dd] = 1.0
    cb = np.stack([ident, np.ones((128, 128), np.float32), perm], axis=1).astype(ml_dtypes.bfloat16)
    return ident, cb


def make_in_maps(inp):
    f = lambda a: np.ascontiguousarray(np.asarray(a, dtype=np.float32))
    x, c, ctx, c_ctx = f(inp["x"]), f(inp["c"]), f(inp["ctx"]), f(inp["c_ctx"])
    w_mod, b_mod = f(inp["w_mod"])[0], f(inp["b_mod"])[0]
    w_in = f(inp["w_in"])[0]
    w_out = f(inp["w_out"])[0]
    w_router = f(inp["w_router"])[0]
    b_router = f(inp["b_router"])[0]
    w_gu, b_gu = f(inp["w_gate_up"])[0], f(inp["b_gate_up"])[0]
    w_dn, b_dn = f(inp["w_down"])[0], f(inp["b_down"])[0]
    ident, cb = _consts()
    chunk = lambda v: np.ascontiguousarray(v.reshape(-1, 128).T)
    gvec = np.zeros((128, 16, 4), np.float32)
    gvec[:, :, 0] = chunk(f(inp["g_norm1"])[0])
    gvec[:, :, 1] = chunk(f(inp["g_norm2"])[0])
    gvec[:, :, 2] = chunk(np.concatenate([f(inp["g_att_out"])[0], f(inp["g_rec_out"])[0]]))
    gqk = np.stack([f(inp["g_q"])[0], f(inp["g_k"])[0]], axis=1)
    bm_row = np.ascontiguousarray(np.broadcast_to(b_mod[None, :], (2, 6 * D)))
    conv_w, conv_b = f(inp["conv_w"])[0], f(inp["conv_b"])[0]
    wa, ba = f(inp["w_gate_a"])[0], f(inp["b_gate_a"])[0]
    wx, bx = f(inp["w_gate_x"])[0], f(inp["b_gate_x"])[0]
    lam = f(inp["lru_lambda"])[0]
    bgu_l = np.ascontiguousarray(b_gu.reshape(NE, 16, 128, 2).transpose(2, 0, 1, 3))
    b_router_b = np.ascontiguousarray(np.broadcast_to(b_router[None, :], (128, NE)))
    maps = []
    for core in range(NCORES):
        b, half = core // 2, core % 2
        xa = x[b] if half == 0 else x[b][::-1]
        cl = ctx[b] if half == 0 else ctx[b][::-1]
        cT = np.stack([chunk(c[b]), chunk(c_ctx)], axis=2)
        dirs = (0, 1) if half == 0 else (1, 0)
        convw = np.zeros((128, 8, 6), np.float32)
        taps = [0, 1, 2, 3] if half == 0 else None
        for n in range(8):
            sl = slice(n * 128, (n + 1) * 128)
            if half == 0:
                convw[:, n, 0:4] = conv_w[:, sl].T
            else:
                convw[:, n, 1:5] = conv_w[::-1, sl].T
            convw[:, n, 5] = conv_b[sl]
        wgate = np.zeros((8, 128, 2, 2, 128), np.float32)
        lruv = np.zeros((128, 2, 8, 5), np.float32)
        for li, dr in enumerate(dirs):
            for n in range(8):
                sl = slice(n * 128, (n + 1) * 128)
                wgate[n, :, li, 0, :] = wa[dr, n]
                wgate[n, :, li, 1, :] = wx[dr, n]
                lruv[:, li, n, 0] = ba[dr, sl]
                lruv[:, li, n, 1] = bx[dr, sl]
                lruv[:, li, n, 2] = lam[dr, sl]
        cosT, sinT = _rope_tables(half)
        maps.append({
            "x_all": np.ascontiguousarray(xa), "ctx_l": np.ascontiguousarray(cl), "cT": np.ascontiguousarray(cT),
            "w_mod": w_mod, "bm_row": bm_row, "gvec": gvec, "w_in": w_in, "gqk": np.ascontiguousarray(gqk),
            "convw": convw, "wgate": wgate, "lruv": lruv, "w_out": w_out, "w_router": w_router,
            "b_router_b": b_router_b, "w_gate_up": w_gu, "bgu_l": bgu_l, "w_down": w_dn, "b_down": b_dn,
            "cosT": cosT, "sinT": sinT, "ident_f": ident, "cb": cb,
        })
    return maps


_NC = None


def kernel(**inputs):
    global _NC
    if _NC is None:
        _NC = build()
    maps = make_in_maps(inputs)
    res = run_bass_kernel_spmd(_NC, maps, core_ids=list(range(NCORES)))
    out = np.zeros((4, SEQ, D), np.float32)
    for core in range(NCORES):
        b, half = core // 2, core % 2
        o = res.results[core]["out"]
        if half == 0:
            out[b, :TOWN] = o
        else:
            out[b, TOWN:] = o[::-1]
    return out
```
